# Optimizing a Trainium2 kernel written in Bass

```python
import math
import jax, jax.numpy as jnp
from jax import lax
import numpy as np

D_MODEL = 1024
BATCH = 8
SEQ = 4096
DEPTH = 2

N_MIXERS = 4
GROUP_WIDTH = D_MODEL // N_MIXERS
HEAD_DIM = 64
N_GROUP_HEADS = GROUP_WIDTH // HEAD_DIM
IN_WIDTH = 9 * GROUP_WIDTH
CONV_WIDTH = 31
POOL_WINDOWS = (2, 4, 8, 16)
POOL_GROUP = GROUP_WIDTH // len(POOL_WINDOWS)
DILATED_PATTERNS = ((128, 1), (512, 4), (2048, 16))
BLOCK = 128
DIFF_QK_DIM = HEAD_DIM // 2
ROPE_THETA = 500000.0
ROPE_FRACTION = 4
D_FF = 2816
N_EXPERTS = 8
TOP_K = 2
D_FF_EXPERT = 3584
N_DENSE = (DEPTH + 1) // 2
N_MOE = DEPTH // 2
NORM_EPS = 1e-5

kernel_name = "hymba_style_four_mixer_hybrid_moe"


def rms_norm(x, g):
    xf = x.astype(jnp.float32)
    y = xf * lax.rsqrt(jnp.mean(xf * xf, axis=-1, keepdims=True) + NORM_EPS)
    return (y * g.astype(jnp.float32)).astype(x.dtype)


def rope_tables(seq, rot_dim):
    pos = jnp.arange(seq, dtype=jnp.float32)
    inv = ROPE_THETA ** (-jnp.arange(0, rot_dim, 2, dtype=jnp.float32) / rot_dim)
    ang = pos[:, None] * inv[None, :]
    return jnp.cos(ang), jnp.sin(ang)


def apply_partial_rope(x, cos, sin):
    rot = 2 * cos.shape[-1]
    half = rot // 2
    xr = x[..., :rot].astype(jnp.float32)
    x1, x2 = xr[..., :half], xr[..., half:]
    c = cos[None, :, None, :]
    s = sin[None, :, None, :]
    out = jnp.concatenate([x1 * c - x2 * s, x2 * c + x1 * s], axis=-1).astype(x.dtype)
    return jnp.concatenate([out, x[..., rot:]], axis=-1)


def conformer_conv(u, dw_w, dw_b, ln_g, ln_b, pw_w):
    a, b = jnp.split(u, 2, axis=-1)
    z = a * jax.nn.sigmoid(b)
    z = lax.conv_general_dilated(
        z, dw_w[:, None, :], window_strides=(1,), padding=[(CONV_WIDTH - 1, 0)],
        dimension_numbers=("NWC", "WIO", "NWC"), feature_group_count=GROUP_WIDTH) + dw_b
    zf = z.astype(jnp.float32)
    mu = jnp.mean(zf, axis=-1, keepdims=True)
    var = jnp.mean(jnp.square(zf - mu), axis=-1, keepdims=True)
    zf = (zf - mu) * lax.rsqrt(var + NORM_EPS) * ln_g.astype(jnp.float32) + ln_b.astype(jnp.float32)
    z = jax.nn.silu(zf).astype(u.dtype)
    return z @ pw_w


def pool_mixer(u, pool_w, pool_scale):
    B, S, _ = u.shape
    uf = u.astype(jnp.float32)
    cs = jnp.cumsum(uf, axis=1)
    pos = jnp.arange(S)
    outs = []
    for g, w in enumerate(POOL_WINDOWS):
        sl = slice(g * POOL_GROUP, (g + 1) * POOL_GROUP)
        c = cs[..., sl]
        lag = jnp.pad(c[:, :-w], ((0, 0), (w, 0), (0, 0)))
        cnt = jnp.minimum(pos + 1, w).astype(jnp.float32)[None, :, None]
        outs.append((c - lag) / cnt - uf[..., sl])
    y = jnp.stack(outs, axis=2).astype(u.dtype)
    y = jnp.einsum("bsgp,gpq->bsgq", y, pool_w).reshape(B, S, GROUP_WIDTH)
    return y * pool_scale


def dilated_branch(q, k, v, dil, span):
    B, S, H, Dh = q.shape
    nb = -(-S // (dil * BLOCK))
    Sp = nb * BLOCK * dil

    def to_blocks(t):
        t = jnp.pad(t, ((0, 0), (0, Sp - S), (0, 0), (0, 0)))
        return t.reshape(B, nb, BLOCK, dil, H, Dh)

    def with_prev(t):
        prev = jnp.pad(t[:, :-1], ((0, 0), (1, 0), (0, 0), (0, 0), (0, 0), (0, 0)))
        return jnp.concatenate([prev, t], axis=2)

    qb = to_blocks(q)
    kk = with_prev(to_blocks(k))
    vv = with_prev(to_blocks(v))
    i = jnp.arange(BLOCK)[:, None]
    j = jnp.arange(2 * BLOCK)[None, :]
    dist = BLOCK + i - j
    band = (dist >= 0) & (dist <= span)
    valid = (jnp.arange(nb)[:, None, None] > 0) | (j >= BLOCK)[None]
    mask = band[None] & valid
    s = jnp.einsum("bnirhd,bnjrhd->bnrhij", qb, kk).astype(jnp.float32) * (Dh ** -0.5)
    s = jnp.where(mask[None, :, None, None], s, -jnp.inf)
    m = jnp.max(s, axis=-1, keepdims=True)
    p = jnp.exp(s - m)
    l = jnp.sum(p, axis=-1)
    o = jnp.einsum("bnrhij,bnjrhd->bnirhd", p.astype(v.dtype), vv).astype(jnp.float32)
    l_t = jnp.transpose(l, (0, 1, 4, 2, 3))
    lse = jnp.transpose(m[..., 0], (0, 1, 4, 2, 3)) + jnp.log(l_t)
    o = (o / l_t[..., None]).reshape(B, Sp, H, Dh)[:, :S]
    lse = lse.reshape(B, Sp, H)[:, :S]
    return o, lse


def dilated_attention(q, k, v):
    outs, lses = [], []
    for window, dil in DILATED_PATTERNS:
        o, lse = dilated_branch(q, k, v, dil, window // dil)
        outs.append(o)
        lses.append(lse)
    wts = jax.nn.softmax(jnp.stack(lses), axis=0)
    out = jnp.sum(wts[..., None] * jnp.stack(outs), axis=0)
    return out.astype(q.dtype)


def diff_attention(q, k, v, lam, lam_init, ln_g):
    B, S, H, Dv = v.shape
    nb = S // BLOCK
    q1, q2 = jnp.split(q, 2, axis=-1)
    k1, k2 = jnp.split(k, 2, axis=-1)
    scale = DIFF_QK_DIM ** -0.5
    q1b = jnp.moveaxis(q1.reshape(B, nb, BLOCK, H, DIFF_QK_DIM), 1, 0)
    q2b = jnp.moveaxis(q2.reshape(B, nb, BLOCK, H, DIFF_QK_DIM), 1, 0)
    kpos = jnp.arange(S)

    def one_block(args):
        n, qb1, qb2 = args
        qpos = n * BLOCK + jnp.arange(BLOCK)
        causal = kpos[None, :] <= qpos[:, None]

        def probs(qb, kf):
            s = jnp.einsum("bihd,bjhd->bhij", qb, kf).astype(jnp.float32) * scale
            return jax.nn.softmax(jnp.where(causal, s, -jnp.inf), axis=-1)

        a = probs(qb1, k1) - lam * probs(qb2, k2)
        return jnp.einsum("bhij,bjhd->bihd", a.astype(v.dtype), v)

    o = lax.map(one_block, (jnp.arange(nb), q1b, q2b))
    o = jnp.moveaxis(o, 0, 1).reshape(B, S, H, Dv)
    o = rms_norm(o, ln_g) * (1.0 - lam_init)
    return o.reshape(B, S, H * Dv)


def token_mixing(h, w_in, dw_w, dw_b, ln_g, ln_b, pw_w, pool_w, pool_scale,
                 lam_vecs, diff_g, w_out, lam_init, cos_c, sin_c, cos_d, sin_d):
    B, S, _ = h.shape
    G = GROUP_WIDTH
    u = h @ w_in
    y_a = conformer_conv(u[..., :2 * G], dw_w, dw_b, ln_g, ln_b, pw_w)
    y_b = pool_mixer(u[..., 2 * G:3 * G], pool_w, pool_scale)
    heads = lambda t: t.reshape(B, S, N_GROUP_HEADS, HEAD_DIM)
    qc = apply_partial_rope(heads(u[..., 3 * G:4 * G]), cos_c, sin_c)
    kc = apply_partial_rope(heads(u[..., 4 * G:5 * G]), cos_c, sin_c)
    vc = heads(u[..., 5 * G:6 * G])
    y_c = dilated_attention(qc, kc, vc).reshape(B, S, G)
    comp = lambda t: t.reshape(B, S, 2 * N_GROUP_HEADS, DIFF_QK_DIM)
    qd = apply_partial_rope(comp(u[..., 6 * G:7 * G]), cos_d, sin_d).reshape(B, S, N_GROUP_HEADS, HEAD_DIM)
    kd = apply_partial_rope(comp(u[..., 7 * G:8 * G]), cos_d, sin_d).reshape(B, S, N_GROUP_HEADS, HEAD_DIM)
    vd = heads(u[..., 8 * G:9 * G])
    lv = lam_vecs.astype(jnp.float32)
    lam = jnp.exp(jnp.sum(lv[0] * lv[1])) - jnp.exp(jnp.sum(lv[2] * lv[3])) + lam_init
    y_d = diff_attention(qd, kd, vd, lam, lam_init, diff_g)
    y = jnp.concatenate([y_a, y_b, y_c, y_d], axis=-1)
    return y @ w_out


def swiglu(h, wg, wu, wd):
    return (jax.nn.silu(h @ wg) * (h @ wu)) @ wd


def moe_swiglu(h, router, wg, wu, wd):
    B, S, D = h.shape
    t = h.reshape(-1, D)
    logits = (t @ router).astype(jnp.float32)
    top_vals, top_idx = lax.top_k(logits, TOP_K)
    gates = jax.nn.softmax(top_vals, axis=-1)
    combine = jnp.sum(jax.nn.one_hot(top_idx, N_EXPERTS, dtype=jnp.float32) * gates[..., None], axis=1)
    y = jnp.zeros(t.shape, jnp.float32)
    for e in range(N_EXPERTS):
        y = y + combine[:, e:e + 1] * swiglu(t, wg[e], wu[e], wd[e]).astype(jnp.float32)
    return y.astype(h.dtype).reshape(B, S, D)


def setup_inputs(seed: int = 0) -> dict:
    key = jax.random.key(seed)
    ks = jax.random.split(key, 24)
    f32 = jnp.float32
    nrm = lambda k, shape, scale: jax.random.normal(k, shape, f32) * scale
    G, L = GROUP_WIDTH, DEPTH
    return {
        "x": jax.random.normal(ks[0], (BATCH, SEQ, D_MODEL), f32),
        "norm1_g": 1.0 + nrm(ks[1], (L, D_MODEL), 0.02),
        "w_in": nrm(ks[2], (L, D_MODEL, IN_WIDTH), D_MODEL ** -0.5),
        "conv_dw_w": nrm(ks[3], (L, CONV_WIDTH, G), CONV_WIDTH ** -0.5),
        "conv_dw_b": nrm(ks[4], (L, G), 0.02),
        "conv_ln_g": 1.0 + nrm(ks[5], (L, G), 0.02),
        "conv_ln_b": nrm(ks[6], (L, G), 0.02),
        "conv_pw_w": nrm(ks[7], (L, G, G), G ** -0.5),
        "pool_w": nrm(ks[8], (L, len(POOL_WINDOWS), POOL_GROUP, POOL_GROUP), POOL_GROUP ** -0.5),
        "pool_scale": 1.0 + nrm(ks[9], (L, G), 0.1),
        "diff_lam": nrm(ks[10], (L, 4, DIFF_QK_DIM), 0.1),
        "diff_ln_g": 1.0 + nrm(ks[11], (L, HEAD_DIM), 0.02),
        "w_out": nrm(ks[12], (L, D_MODEL, D_MODEL), D_MODEL ** -0.5),
        "norm2_g": 1.0 + nrm(ks[13], (L, D_MODEL), 0.02),
        "ffn_w_gate": nrm(ks[14], (N_DENSE, D_MODEL, D_FF), D_MODEL ** -0.5),
        "ffn_w_up": nrm(ks[15], (N_DENSE, D_MODEL, D_FF), D_MODEL ** -0.5),
        "ffn_w_down": nrm(ks[16], (N_DENSE, D_FF, D_MODEL), D_FF ** -0.5),
        "moe_router": nrm(ks[17], (N_MOE, D_MODEL, N_EXPERTS), D_MODEL ** -0.5),
        "moe_w_gate": nrm(ks[18], (N_MOE, N_EXPERTS, D_MODEL, D_FF_EXPERT), D_MODEL ** -0.5),
        "moe_w_up": nrm(ks[19], (N_MOE, N_EXPERTS, D_MODEL, D_FF_EXPERT), D_MODEL ** -0.5),
        "moe_w_down": nrm(ks[20], (N_MOE, N_EXPERTS, D_FF_EXPERT, D_MODEL), D_FF_EXPERT ** -0.5),
        "final_g": 1.0 + nrm(ks[21], (D_MODEL,), 0.02),
    }


def reference(x, norm1_g, w_in, conv_dw_w, conv_dw_b, conv_ln_g, conv_ln_b, conv_pw_w,
              pool_w, pool_scale, diff_lam, diff_ln_g, w_out, norm2_g,
              ffn_w_gate, ffn_w_up, ffn_w_down, moe_router, moe_w_gate, moe_w_up, moe_w_down,
              final_g):
    S = x.shape[1]
    cos_c, sin_c = rope_tables(S, HEAD_DIM // ROPE_FRACTION)
    cos_d, sin_d = rope_tables(S, DIFF_QK_DIM // ROPE_FRACTION)
    for layer in range(DEPTH):
        lam_init = 0.8 - 0.6 * math.exp(-0.3 * layer)
        h = rms_norm(x, norm1_g[layer])
        x = x + token_mixing(h, w_in[layer], conv_dw_w[layer], conv_dw_b[layer], conv_ln_g[layer],
                             conv_ln_b[layer], conv_pw_w[layer], pool_w[layer], pool_scale[layer],
                             diff_lam[layer], diff_ln_g[layer], w_out[layer], lam_init,
                             cos_c, sin_c, cos_d, sin_d)
        h = rms_norm(x, norm2_g[layer])
        if layer % 2 == 0:
            i = layer // 2
            x = x + swiglu(h, ffn_w_gate[i], ffn_w_up[i], ffn_w_down[i])
        else:
            i = layer // 2
            x = x + moe_swiglu(h, moe_router[i], moe_w_gate[i], moe_w_up[i], moe_w_down[i])
    return rms_norm(x, final_g)
```

```python
import math
from contextlib import ExitStack
import numpy as np
import concourse.bass as bass
import concourse.mybir as mybir
from concourse.bass_utils import run_bass_kernel_spmd

F32 = mybir.dt.float32
BF16 = mybir.dt.bfloat16
AF = mybir.ActivationFunctionType
ALU = mybir.AluOpType
AX = mybir.AxisListType

S = 4096
D = 1024
G = 256
NT = 32
DFF = 2816
DFE = 3584
NE = 8
EPS = 1e-5
THETA = 500000.0


import os
ATTACH_WAITS = os.environ.get('KATTACH', '1') == '1'


class Buf:
    __slots__ = ("w", "r")

    def __init__(self):
        self.w = None
        self.r = {}


def bufs(n):
    return [Buf() for _ in range(n)]


class Eng:
    def __init__(self, name, sem):
        self.name = name
        self.sem = sem
        self.count = 0
        self.ops = []
        self.waited = {}
        self.pend_r = []
        self.pend_w = []


class Prog:
    def __init__(self, nc):
        self.nc = nc
        self.sems = {}
        self.semcount = {}
        self.E = {}
        for n in ("pe", "act", "dve", "pool", "sp"):
            h = nc.alloc_semaphore("s_" + n)
            self.sems[n] = h
            self.E[n] = Eng(n, h)
        self.nops = 0
        self.free_dma = {}
        self.gen = 0
        self.all_dma = []
        self.keep = []

    def _wait(self, e, key, val, acc=None):
        if e.name == "pe" and key == "pe":
            return
        if key not in self.sems:
            return
        if e.waited.get(key, 0) >= val:
            return
        e.waited[key] = val
        h = self.sems[key]
        if acc is not None:
            acc.append((h, val))
        else:
            e.ops.append(lambda eh, h=h, val=val: eh.wait_ge(h, val))

    def _deps(self, e, reads, writes, acc=None):
        for b in reads:
            if b.w is not None:
                self._wait(e, *b.w, acc=acc)
        for b in writes:
            if b.w is not None:
                self._wait(e, *b.w, acc=acc)
            for k, v in b.r.items():
                self._wait(e, k, v, acc=acc)

    def op(self, eng, fn, reads=(), writes=(), inc=True):
        e = self.E[eng]
        acc = [] if ATTACH_WAITS else None
        self._deps(e, reads, writes, acc)
        att = None
        if acc:
            att = acc.pop()
            for (h, val) in acc:
                e.ops.append(lambda eh, h=h, val=val: eh.wait_ge(h, val))
        e.pend_r.extend(reads)
        e.pend_w.extend(writes)
        self.nops += 1

        def emit(eh, fn=fn, att=att):
            ins = fn(eh)
            if att is not None:
                ins = ins._wait_ge(att[0], att[1])
            return ins
        if inc:
            e.count += 1
            ev = (eng, e.count)
            h = e.sem
            e.ops.append(lambda eh, emit=emit, h=h: emit(eh).then_inc(h, 1))
            for b in e.pend_w:
                b.w = ev
                b.r = {}
            for b in e.pend_r:
                if b.w != ev:
                    b.r[eng] = e.count
            e.pend_r = []
            e.pend_w = []
        else:
            e.ops.append(emit)

    def dma(self, queue, out, in_, reads=(), writes=(), semkey=None):
        e = self.E[queue]
        self._deps(e, reads, writes)
        b0 = writes[0] if len(writes) else reads[0]
        semkey = (id(b0), queue, self.gen)
        if semkey not in self.sems:
            if self.free_dma.get(queue):
                h, c = self.free_dma[queue].pop()
            else:
                h, c = self.nc.alloc_semaphore(f"d{len(self.all_dma)}"), 0
                self.all_dma.append(h)
            self.sems[semkey] = h
            self.semcount[semkey] = c
            self.keep.append(b0)
        self.semcount[semkey] += 16
        val = self.semcount[semkey]
        h = self.sems[semkey]
        e.ops.append(lambda eh, out=out, in_=in_, h=h: eh.dma_start(out=out, in_=in_).then_inc(h, 16))
        for b in writes:
            b.w = (semkey, val)
            b.r = {}
        for b in reads:
            b.r[semkey] = val
        self.nops += 1

    def barrier(self):
        snap = dict(self.semcount)
        for n, e in self.E.items():
            assert not e.pend_r and not e.pend_w, n
            snap[n] = e.count
        for n, e in self.E.items():
            for k, v in snap.items():
                if v > 0 and k != n:
                    self._wait(e, k, v)
        self.gen += 1
        for k in list(self.semcount.keys()):
            self.free_dma.setdefault(k[1], []).append((self.sems[k], self.semcount[k]))
            del self.sems[k]
            del self.semcount[k]
            for e in self.E.values():
                e.waited.pop(k, None)

    def flush(self):
        nc = self.nc
        E = self.E
        with nc.Block() as block:
            @block.sync
            def _(eh):
                for f in E["sp"].ops:
                    f(eh)

            @block.tensor
            def _(eh):
                for f in E["pe"].ops:
                    f(eh)

            @block.scalar
            def _(eh):
                for f in E["act"].ops:
                    f(eh)

            @block.vector
            def _(eh):
                for f in E["dve"].ops:
                    f(eh)

            @block.gpsimd
            def _(eh):
                for f in E["pool"].ops:
                    f(eh)
        for e in E.values():
            e.ops = []


class Scope:
    cnt = [0]

    def __init__(self, nc):
        self.nc = nc
        self.st = ExitStack()

    def sb(self, name, shape, dt):
        Scope.cnt[0] += 1
        return self.st.enter_context(self.nc.sbuf_tensor(f"{name}_{Scope.cnt[0]}", list(shape), dt)).ap()

    def ps(self, name, shape, dt=F32):
        Scope.cnt[0] += 1
        return self.st.enter_context(self.nc.psum_tensor(f"{name}_{Scope.cnt[0]}", list(shape), dt)).ap()

    def close(self):
        self.st.close()


class Rot:
    def __init__(self, items):
        self.items = items
        self.i = 0

    def next(self):
        it = self.items[self.i % len(self.items)]
        self.i += 1
        return it


def build_program(debug=False, only=None, nlayers=2):
    nc = bass.Bass("TRN2", target_bir_lowering=False)
    P = Prog(nc)

    def din(name, shape, dt=F32):
        return nc.dram_tensor(name, list(shape), dt, kind="ExternalInput").ap()

    def dscr(name, shape, dt):
        return nc.dram_tensor(name, list(shape), dt, kind=("ExternalOutput" if debug else "Internal")).ap()

    x_in = din("x", [S, D])
    norm1_g = din("norm1_g", [2, D])
    w_in = din("w_in", [2, D, 9 * G])
    w_inP = din("w_inP", [2, D, 4 * G])
    dwcol = din("dwcol", [2, 128, 2 * 31])
    cvec = din("cvec", [2, 128, 16])
    conv_pw_w = din("conv_pw_w", [2, G, G])
    pool_w = din("pool_w", [2, 4, 64, 64])
    diff_lam = din("diff_lam", [2, 1, 128])
    w_out = din("w_out", [2, D, D])
    norm2_g = din("norm2_g", [2, D])
    ffn_wg = din("ffn_w_gate", [1, D, DFF])
    ffn_wu = din("ffn_w_up", [1, D, DFF])
    ffn_wd = din("ffn_w_down", [1, DFF, D])
    routerT = din("routerT", [1, NE * D])
    moe_wg = din("moe_w_gate", [1, NE, D, DFE])
    moe_wu = din("moe_w_up", [1, NE, D, DFE])
    moe_wd = din("moe_w_down", [1, NE, DFE, D])
    final_g = din("final_g", [1, D])
    ropeT = din("ropeT", [4, 128, S])
    cst = din("cst", [128, 128 * 3 + 512 + 32])
    out = nc.dram_tensor("out", [S, D], F32, kind="ExternalOutput").ap()

    zA_d = dscr("zA_d", [2 * 128, S], BF16)
    uB_d = dscr("uB_d", [2 * 128, S], F32)
    qk_d = dscr("qk_d", [16 * 128, S], BF16)
    v_d = dscr("v_d", [S, 512], BF16)
    yT_d = dscr("yT_d", [D, S], BF16)
    xmid = dscr("xmid", [S, D], F32)

    gs = Scope(nc)
    ident = gs.sb("ident", [128, 128], BF16)
    ones_bf = gs.sb("ones_bf", [128, 128], BF16)
    blockones = gs.sb("blockones", [128, 128], F32)
    ones_f = gs.sb("ones_f", [128, 128], F32)
    mask = gs.sb("mask", [128, 512], BF16)
    pooltab = gs.sb("pooltab", [128, 2, 16], F32)
    Bc = Buf()
    P.dma("pool", ident, cst[:, 0:128], writes=[Bc], semkey="c")
    P.dma("pool", blockones, cst[:, 128:256], writes=[Bc], semkey="c")
    P.dma("pool", ones_f, cst[:, 256:384], writes=[Bc], semkey="c")
    P.dma("pool", ones_bf, cst[:, 256:384], writes=[Bc], semkey="c")
    P.dma("pool", mask, cst[:, 384:896], writes=[Bc], semkey="c")
    P.dma("pool", pooltab, cst[:, 896:928].rearrange("p (c t) -> p c t", c=2), writes=[Bc], semkey="c")

    def rstd_ops(stat, Bst, n_inv, src, Bsrc, junk, Bjunk):
        P.op("act", lambda e: e.activation(out=junk, in_=src, func=AF.Square, accum_out=stat[:, 0:1]),
             reads=[Bsrc], writes=[Bjunk, Bst])
        P.op("dve", lambda e: e.tensor_scalar(out=stat[:, 1:2], in0=stat[:, 0:1], scalar1=n_inv, scalar2=EPS,
                                              op0=ALU.mult, op1=ALU.add), reads=[Bst], writes=[Bst])
        P.op("act", lambda e: e.activation(out=stat[:, 1:2], in_=stat[:, 1:2], func=AF.Sqrt), reads=[Bst], writes=[Bst])
        P.op("dve", lambda e: e.reciprocal(out=stat[:, 1:2], in_=stat[:, 1:2]), reads=[Bst], writes=[Bst])

    def phase_p1(l, x_src):
        sc = Scope(nc)
        w_sb = sc.sb("w_in", [128, 8, 3328], BF16)
        Bw = Buf()
        for k in range(8):
            P.dma("pool", w_sb[:, k, 0:2304], w_in[l, k * 128:(k + 1) * 128, :], writes=[Bw], semkey="w")
            P.dma("pool", w_sb[:, k, 2304:3328], w_inP[l, k * 128:(k + 1) * 128, :], writes=[Bw], semkey="w")
        g1 = sc.sb("g1", [128, D], F32)
        cv = sc.sb("cv", [128, 16], F32)
        Bg = Buf()
        P.dma("sp", g1, norm1_g[l:l + 1, :].partition_broadcast(128), writes=[Bg], semkey="c")
        P.dma("sp", cv, cvec[l], writes=[Bg], semkey="c")
        xs = [(sc.sb("xs", [128, 4, D], F32), Buf()) for _ in range(2)]
        rp = [(sc.sb("rope", [128, 4, 512], F32), Buf()) for _ in range(2)]
        hT = [(sc.sb("hT", [128, 8, 512], BF16), Buf()) for _ in range(2)]
        zst = [(sc.sb("zst", [128, 2, 512], BF16), Buf()) for _ in range(2)]
        ust = [(sc.sb("ust", [128, 2, 512], F32), Buf()) for _ in range(2)]
        qst = [(sc.sb("qst", [128, 16, 512], BF16), Buf()) for _ in range(2)]
        vst = [(sc.sb("vst", [128, 4, 512], BF16), Buf()) for _ in range(2)]
        hbf = Rot([(sc.sb("hbf", [128, D], BF16), Buf()) for _ in range(5)])
        tmpf = Rot([(sc.sb("tmpf", [128, 512], F32), Buf()) for _ in range(6)])
        junk = sc.sb("junk", [128, D], BF16)
        Bj = Buf()
        stat = sc.sb("stat", [128, 8, 2], F32)
        Bst = bufs(8)
        pT = Rot([(sc.ps("pT", [128, 8, 128], BF16), Buf()) for _ in range(2)])
        pp = Rot([(sc.ps("pp", [128, 512], F32), Buf()) for _ in range(6)])

        def proj(col0, hTt, BhT):
            p, Bp = pp.next()
            for k in range(8):
                P.op("pe", lambda e, p=p, k=k: e.matmul(p, lhsT=w_sb[:, k, col0:col0 + 128], rhs=hTt[:, k, :],
                                                         start=(k == 0), stop=(k == 7)),
                     reads=[Bw, BhT], writes=[Bp], inc=(k == 7))
            return p, Bp

        hbs = {}

        def n_stage(ci):
            pb = ci % 2
            xt, Bx = xs[pb]
            rt, Br = rp[pb]
            P.dma("sp", xt, x_src[ci * 512:(ci + 1) * 512, :].rearrange("(t p) d -> p t d", p=128), writes=[Bx])
            P.dma("sp", rt, ropeT[:, :, ci * 512:(ci + 1) * 512].rearrange("f p t -> p f t"), writes=[Br])
            for t in range(4):
                si = (ci * 4 + t) % 8
                st = stat[:, si, :]
                rstd_ops(st, Bst[si], 1.0 / D, xt[:, t, :], Bx, junk, Bj)
                hb, Bh = hbf.next()
                hbs[(ci, t)] = (hb, Bh)
                P.op("dve", lambda e, hb=hb, t=t, st=st, xt=xt: e.scalar_tensor_tensor(
                    out=hb, in0=xt[:, t, :], scalar=st[:, 1:2], in1=g1, op0=ALU.mult, op1=ALU.mult),
                    reads=[Bx, Bst[si], Bg], writes=[Bh])

        def t_stage(ci):
            pb = ci % 2
            hTt, BhT = hT[pb]
            for t in range(4):
                hb, Bh = hbs.pop((ci, t))
                pt_, Bpt = pT.next()
                for k in range(8):
                    P.op("pe", lambda e, pt_=pt_, hb=hb, k=k: e.transpose(pt_[:, k, :], hb[:, k * 128:(k + 1) * 128], ident),
                         reads=[Bh, Bc], writes=[Bpt], inc=(k == 7))
                P.op("act", lambda e, pt_=pt_, t=t, hTt=hTt: e.copy(out=hTt[:, :, t * 128:(t + 1) * 128], in_=pt_),
                     reads=[Bpt], writes=[BhT])

        def j_stage(ci):
            pb = ci % 2
            rt, Br = rp[pb]
            hTt, BhT = hT[pb]
            zt, Bz = zst[pb]
            ut, Bu = ust[pb]
            qt, Bq = qst[pb]
            vt, Bv = vst[pb]
            for cc in range(2):
                pa, Bpa = proj(cc * 128, hTt, BhT)
                pbb, Bpb = proj(256 + cc * 128, hTt, BhT)
                tf, Btf = tmpf.next()
                P.op("act", lambda e, tf=tf, pbb=pbb: e.activation(out=tf, in_=pbb, func=AF.Sigmoid), reads=[Bpb], writes=[Btf])
                P.op("dve", lambda e, zt=zt, cc=cc, pa=pa, tf=tf: e.tensor_tensor(out=zt[:, cc, :], in0=pa, in1=tf, op=ALU.mult),
                     reads=[Bpa, Btf], writes=[Bz])
            for cc in range(2):
                p_, Bp_ = proj(512 + cc * 128, hTt, BhT)
                P.op("act", lambda e, ut=ut, cc=cc, p_=p_: e.copy(out=ut[:, cc, :], in_=p_), reads=[Bp_], writes=[Bu])
            specs = []
            for cc in range(2):
                specs.append((768 + cc * 128, 2304 + cc * 128, 0, [(cc, None)]))
                specs.append((1024 + cc * 128, 2560 + cc * 128, 0, [(2 + cc, 13), (4 + cc, 14)]))
                specs.append((1536 + cc * 128, 2816 + cc * 128, 2, [(6 + cc, None)]))
                specs.append((1792 + cc * 128, 3072 + cc * 128, 2, [(8 + cc, 9), (10 + cc, 10), (12 + cc, 11), (14 + cc, 12)]))
            for col, colP, ri, slots in specs:
                pu, Bpu = proj(col, hTt, BhT)
                ppm, Bppm = proj(colP, hTt, BhT)
                t1, Bt1 = tmpf.next()
                t2, Bt2 = tmpf.next()
                P.op("dve", lambda e, t1=t1, pu=pu, ri=ri, rt=rt: e.tensor_tensor(out=t1, in0=pu, in1=rt[:, ri, :], op=ALU.mult),
                     reads=[Bpu, Br], writes=[Bt1])
                P.op("dve", lambda e, t2=t2, ppm=ppm, ri=ri, rt=rt: e.tensor_tensor(out=t2, in0=ppm, in1=rt[:, ri + 1, :], op=ALU.mult),
                     reads=[Bppm, Br], writes=[Bt2])
                if slots[0][1] is None:
                    s0 = slots[0][0]
                    P.op("dve", lambda e, qt=qt, s0=s0, t1=t1, t2=t2: e.tensor_tensor(out=qt[:, s0, :], in0=t1, in1=t2, op=ALU.add),
                         reads=[Bt1, Bt2], writes=[Bq])
                else:
                    P.op("dve", lambda e, t1=t1, t2=t2: e.tensor_tensor(out=t1, in0=t1, in1=t2, op=ALU.add),
                         reads=[Bt1, Bt2], writes=[Bt1])
                    for s0, mc in slots:
                        P.op("act", lambda e, qt=qt, s0=s0, t1=t1, mc=mc: e.activation(
                            out=qt[:, s0, :], in_=t1, func=AF.Identity, scale=cv[:, mc:mc + 1]),
                            reads=[Bt1, Bg], writes=[Bq])
            for t in range(4):
                p_, Bp_ = pp.next()
                for half, c0 in enumerate((1280, 2048)):
                    for k in range(8):
                        P.op("pe", lambda e, p_=p_, half=half, c0=c0, k=k, t=t, hTt=hTt: e.matmul(
                            p_[:, half * 256:(half + 1) * 256], lhsT=hTt[:, k, t * 128:(t + 1) * 128],
                            rhs=w_sb[:, k, c0:c0 + 256], start=(k == 0), stop=(k == 7)),
                            reads=[Bw, BhT], writes=[Bp_], inc=(half == 1 and k == 7))
                P.op("act", lambda e, vt=vt, t=t, p_=p_: e.copy(out=vt[:, t, :], in_=p_), reads=[Bp_], writes=[Bv])
            cs = slice(ci * 512, (ci + 1) * 512)
            P.dma("sp", zA_d.rearrange("(c p) t -> p c t", p=128)[:, :, cs], zt, reads=[Bz], semkey=f"st{pb}")
            P.dma("sp", uB_d.rearrange("(c p) t -> p c t", p=128)[:, :, cs], ut, reads=[Bu], semkey=f"st{pb}")
            P.dma("sp", qk_d.rearrange("(c p) t -> p c t", p=128)[:, :, cs], qt, reads=[Bq], semkey=f"st{pb}")
            P.dma("sp", v_d[cs, :].rearrange("(t p) c -> p t c", p=128), vt, reads=[Bv], semkey=f"st{pb}")
        n_stage(0)
        t_stage(0)
        for ci in range(8):
            if ci + 1 < 8:
                n_stage(ci + 1)
            j_stage(ci)
            if ci + 1 < 8:
                t_stage(ci + 1)
        P.barrier()
        P.flush()
        sc.close()

    def phase_conv(l):
        sc = Scope(nc)
        zp = sc.sb("zp", [128, 2, 30 + S], BF16)
        Bz = Buf()
        P.op("dve", lambda e: e.memset(zp[:, :, 0:30], 0.0), writes=[Bz])
        P.dma("sp", zp[:, :, 30:30 + S], zA_d.rearrange("(c p) t -> p c t", p=128), writes=[Bz], semkey="ld0")
        dwc = sc.sb("dwc", [128, 2, 31], F32)
        cv = sc.sb("cv", [128, 16], F32)
        pw = sc.sb("pw", [128, 2, G], BF16)
        Bk = Buf()
        P.dma("sp", dwc, dwcol[l].rearrange("p (c j) -> p c j", c=2), writes=[Bk], semkey="ld1")
        P.dma("sp", cv, cvec[l], writes=[Bk], semkey="ld1")
        Bpw = Buf()
        P.dma("pool", pw, conv_pw_w[l].rearrange("(c p) g -> p c g", p=128), writes=[Bpw], semkey="ld2")
        dg = sc.sb("dg", [128, 2, 31, 128], BF16)
        Bdg = Buf()
        for cc in range(2):
            for j in range(31):
                P.op("pool", lambda e, cc=cc, j=j: e.tensor_scalar(out=dg[:, cc, j, :], in0=ident, scalar1=dwc[:, cc, j:j + 1],
                                                                   scalar2=None, op0=ALU.mult), reads=[Bc, Bk], writes=[Bdg])
        pc = [[(sc.ps("pc", [128, 512]), Buf()) for _ in range(2)] for _ in range(2)]
        pS1, BS1 = sc.ps("pS1", [128, 512]), Buf()
        pS2, BS2 = sc.ps("pS2", [128, 512]), Buf()
        py = Rot([(sc.ps("py", [128, 512]), Buf()) for _ in range(2)])
        cvs2 = [[(sc.sb("cvs", [128, 512], F32), Buf()) for _ in range(2)] for _ in range(2)]
        sqs2 = [[(sc.sb("sqs", [128, 512], F32), Buf()) for _ in range(2)] for _ in range(2)]
        mean, Bm = sc.sb("mean", [128, 512], F32), Buf()
        msq, Bmsq = sc.sb("msq", [128, 512], F32), Buf()
        var, Bvar = sc.sb("var", [128, 512], F32), Buf()
        dd = [(sc.sb("dd", [128, 512], F32), Buf()) for _ in range(2)]
        zs = [(sc.sb("zs", [128, 512], BF16), Buf()) for _ in range(2)]
        yst = Rot([(sc.sb("yst", [128, 2, 512], BF16), Buf()) for _ in range(2)])
        def c1_stage(ci):
            cvs = cvs2[ci % 2]
            sqs = sqs2[ci % 2]
            for cc in range(2):
                p_, Bp_ = pc[ci % 2][cc]
                for j in range(31):
                    P.op("pe", lambda e, p_=p_, cc=cc, j=j, ci=ci: e.matmul(
                        p_, lhsT=dg[:, cc, j, :], rhs=zp[:, cc, ci * 512 + j: ci * 512 + j + 512],
                        start=(j == 0), stop=(j == 30)), reads=[Bdg, Bz], writes=[Bp_], inc=(j == 30))
                c_, Bc_ = cvs[cc]
                s_, Bs_ = sqs[cc]
                P.op("act", lambda e, c_=c_, p_=p_, cc=cc: e.activation(out=c_, in_=p_, func=AF.Identity, bias=cv[:, cc:cc + 1]),
                     reads=[Bp_, Bk], writes=[Bc_])
                P.op("act", lambda e, s_=s_, p_=p_, cc=cc: e.activation(out=s_, in_=p_, func=AF.Square, bias=cv[:, cc:cc + 1]),
                     reads=[Bp_, Bk], writes=[Bs_])
        def c2_stage(ci):
            cvs = cvs2[ci % 2]
            sqs = sqs2[ci % 2]
            for cc in range(2):
                P.op("pe", lambda e, cc=cc: e.matmul(pS1, lhsT=ones_f, rhs=cvs[cc][0], start=(cc == 0), stop=(cc == 1)),
                     reads=[Bc, cvs[cc][1]], writes=[BS1], inc=(cc == 1))
            for cc in range(2):
                P.op("pe", lambda e, cc=cc: e.matmul(pS2, lhsT=ones_f, rhs=sqs[cc][0], start=(cc == 0), stop=(cc == 1)),
                     reads=[Bc, sqs[cc][1]], writes=[BS2], inc=(cc == 1))
            P.op("dve", lambda e: e.tensor_scalar(out=mean, in0=pS1, scalar1=1.0 / G, scalar2=None, op0=ALU.mult), reads=[BS1], writes=[Bm])
            P.op("dve", lambda e: e.tensor_tensor(out=msq, in0=mean, in1=mean, op=ALU.mult), reads=[Bm], writes=[Bmsq])
            P.op("dve", lambda e: e.scalar_tensor_tensor(out=var, in0=pS2, scalar=1.0 / G, in1=msq, op0=ALU.mult, op1=ALU.subtract),
                 reads=[BS2, Bmsq], writes=[Bvar])
            P.op("dve", lambda e: e.tensor_scalar(out=var, in0=var, scalar1=EPS, scalar2=None, op0=ALU.add), reads=[Bvar], writes=[Bvar])
            P.op("act", lambda e: e.activation(out=var, in_=var, func=AF.Sqrt), reads=[Bvar], writes=[Bvar])
            P.op("dve", lambda e: e.reciprocal(out=var, in_=var), reads=[Bvar], writes=[Bvar])
            for cc in range(2):
                d_, Bd_ = dd[cc]
                z_, Bz_ = zs[cc]
                P.op("dve", lambda e, d_=d_, cc=cc: e.tensor_tensor(out=d_, in0=cvs[cc][0], in1=mean, op=ALU.subtract),
                     reads=[cvs[cc][1], Bm], writes=[Bd_])
                P.op("dve", lambda e, d_=d_: e.tensor_tensor(out=d_, in0=d_, in1=var, op=ALU.mult), reads=[Bd_, Bvar], writes=[Bd_])
                P.op("act", lambda e, d_=d_, z_=z_, cc=cc: e.activation(out=z_, in_=d_, func=AF.Silu, scale=cv[:, 2 + cc:3 + cc],
                                                                       bias=cv[:, 4 + cc:5 + cc]), reads=[Bd_, Bk], writes=[Bz_])
            ys, Bys = yst.next()
            for go in range(2):
                p_, Bp_ = py.next()
                for cc in range(2):
                    P.op("pe", lambda e, p_=p_, go=go, cc=cc: e.matmul(p_, lhsT=pw[:, cc, go * 128:(go + 1) * 128], rhs=zs[cc][0],
                                                                       start=(cc == 0), stop=(cc == 1)),
                         reads=[Bpw, zs[cc][1]], writes=[Bp_], inc=(cc == 1))
                P.op("act", lambda e, ys=ys, go=go, p_=p_: e.copy(out=ys[:, go, :], in_=p_), reads=[Bp_], writes=[Bys])
            P.dma("sp", yT_d.rearrange("(c p) t -> p c t", p=128)[:, 0:2, ci * 512:(ci + 1) * 512], ys, reads=[Bys], semkey=f"st{ci % 2}")

        c1_stage(0)
        for ci in range(8):
            if ci + 1 < 8:
                c1_stage(ci + 1)
            c2_stage(ci)
        P.barrier()
        P.flush()
        sc.close()

    def phase_pool(l):
        sc = Scope(nc)
        up = sc.sb("up", [128, 2, 16 + S], F32)
        Bu = Buf()
        P.op("dve", lambda e: e.memset(up[:, :, 0:16], 0.0), writes=[Bu])
        P.dma("sp", up[:, :, 16:16 + S], uB_d.rearrange("(c p) t -> p c t", p=128), writes=[Bu], semkey="ld0")
        A, BA = sc.sb("A", [128, 16 + S], F32), Buf()
        Bt, BB = sc.sb("Bt", [128, 16 + S], F32), Buf()
        P.op("dve", lambda e: e.memset(A[:, 0:16], 0.0), writes=[BA])
        P.op("dve", lambda e: e.memset(Bt[:, 0:16], 0.0), writes=[BB])
        ybf, By = sc.sb("ybf", [128, 2, S], BF16), Buf()
        t16, Bt16 = sc.sb("t16", [128, 16], F32), Buf()
        bd, Bbd = sc.sb("bd", [128, 2, 128], BF16), Buf()
        cv = sc.sb("cv", [128, 16], F32)
        Bk = Buf()
        P.dma("sp", cv, cvec[l], writes=[Bk], semkey="ld1")
        P.op("pool", lambda e: e.memset(bd, 0.0), writes=[Bbd])
        for g in range(4):
            o = (g % 2) * 64
            P.dma("pool", bd[o:o + 64, g // 2, o:o + 64], pool_w[l, g], writes=[Bbd], semkey="ld2")

        def shadd(dst, Bd, src, Bs, sh):
            P.op("dve", lambda e: e.tensor_tensor(out=dst[:, 16:16 + S], in0=src[:, 16:16 + S], in1=src[:, 16 - sh:16 - sh + S], op=ALU.add),
                 reads=[Bs], writes=[Bd])

        for cc in range(2):
            u = up[:, cc, :]
            shadd(A, BA, u, Bu, 1)
            shadd(Bt, BB, A, BA, 2)
            if cc == 0:
                wl, wh = 2, 4
            else:
                shadd(A, BA, Bt, BB, 4)
                shadd(Bt, BB, A, BA, 8)
                wl, wh = 8, 16
            for (r0, Ssrc, Bs, w) in ((0, A, BA, wl), (64, Bt, BB, wh)):
                rows = slice(r0, r0 + 64)
                P.op("dve", lambda e, rows=rows, Ssrc=Ssrc, w=w, u=u, cc=cc: e.scalar_tensor_tensor(
                    out=ybf[rows, cc, :], in0=Ssrc[rows, 16:16 + S], scalar=1.0 / w, in1=u[rows, 16:16 + S],
                    op0=ALU.mult, op1=ALU.subtract), reads=[Bs, Bu], writes=[By])
                P.op("dve", lambda e, rows=rows, Ssrc=Ssrc, cc=cc: e.tensor_tensor(
                    out=t16[rows, :], in0=Ssrc[rows, 16:32], in1=pooltab[rows, cc, :], op=ALU.mult), reads=[Bs, Bc], writes=[Bt16])
                P.op("dve", lambda e, rows=rows, u=u, cc=cc: e.tensor_tensor(
                    out=ybf[rows, cc, 0:16], in0=t16[rows, :], in1=u[rows, 16:32], op=ALU.subtract), reads=[Bt16, Bu], writes=[By])
        pq = Rot([(sc.ps("pq", [128, 512]), Buf()) for _ in range(4)])
        yst = Rot([(sc.sb("yst", [128, 2, 512], BF16), Buf()) for _ in range(2)])
        for ci in range(8):
            ys, Bys = yst.next()
            for qo in range(2):
                p_, Bp_ = pq.next()
                P.op("pe", lambda e, p_=p_, qo=qo, ci=ci: e.matmul(p_, lhsT=bd[:, qo, :], rhs=ybf[:, qo, ci * 512:(ci + 1) * 512],
                                                                   start=True, stop=True), reads=[Bbd, By], writes=[Bp_])
                P.op("act", lambda e, ys=ys, qo=qo, p_=p_: e.activation(out=ys[:, qo, :], in_=p_, func=AF.Identity,
                                                                       scale=cv[:, 6 + qo:7 + qo]), reads=[Bp_, Bk], writes=[Bys])
            P.dma("sp", yT_d.rearrange("(c p) t -> p c t", p=128)[:, 2:4, ci * 512:(ci + 1) * 512], ys, reads=[Bys], semkey=f"st{ci % 2}")
        P.barrier()
        P.flush()
        sc.close()

    def phase_dil(l):
        sc = Scope(nc)
        qT, Bq = sc.sb("qT", [128, 2, S], BF16), Buf()
        qkv = qk_d.rearrange("(c p) t -> p c t", p=128)
        P.dma("sp", qT, qkv[:, 0:2, :], writes=[Bq], semkey="ld0")
        kTs = []
        for hh_ in range(2):
            kT_, Bk_ = sc.sb("kT", [128, 2, S], BF16), Buf()
            P.dma("sp", kT_, qkv[:, 2 + 2 * hh_:4 + 2 * hh_, :], writes=[Bk_], semkey="ld0")
            kTs.append((kT_, Bk_))
        vs, Bvs = sc.sb("vs", [128, 32, 256], BF16), Buf()
        vE, BvE = sc.sb("vE", [128, 32, 4, 128], BF16), Buf()
        P.op("dve", lambda e: e.memset(vE, 1.0), writes=[BvE])
        Oacc = sc.sb("Oacc", [128, 4, S], F32)
        BO = bufs(2)
        ps_s = Rot([(sc.ps("ps_s", [128, 512]), Buf()) for _ in range(4)])
        ps_o = Rot([(sc.ps("ps_o", [128, 512]), Buf()) for _ in range(3)])
        es = Rot([(sc.sb("es", [128, 512], BF16), Buf()) for _ in range(6)])
        LA = 3
        dslot = {}

        def dA(i, d, r, n, h, nb):
            c, hh = h // 2, h % 2
            rows = slice(0, 128)
            kT, Bk = kTs[hh]
            qv = qT[:, c, :].rearrange("p (n i dd) -> p n i dd", i=128, dd=d)
            kv = kT[:, c, :].rearrange("p (n i dd) -> p n i dd", i=128, dd=d)
            s_, Bs_ = ps_s.next()
            e_, Be_ = es.next()
            dslot[i] = (e_, Be_)
            W = 256 if n > 0 else 128
            P.op("pe", lambda e: e.matmul(s_[:, 0:128], lhsT=kv[rows, n, :, r], rhs=qv[rows, n, :, r], start=True, stop=True),
                 reads=[Bq, Bk], writes=[Bs_], inc=(n == 0))
            if n > 0:
                P.op("pe", lambda e: e.matmul(s_[:, 128:256], lhsT=kv[rows, n - 1, :, r], rhs=qv[rows, n, :, r], start=True, stop=True),
                     reads=[Bq, Bk], writes=[Bs_])
            P.op("act", lambda e: e.activation(out=e_[:, 0:W], in_=s_[:, 0:W], func=AF.Exp, scale=0.125), reads=[Bs_], writes=[Be_])
            P.op("dve", lambda e: e.tensor_tensor(out=e_[:, 0:W], in0=e_[:, 0:W], in1=mask[:, 0:W], op=ALU.mult),
                 reads=[Be_, Bc], writes=[Be_])

        def dB(i, d, r, n, h, nb):
            c = h // 2
            blk = r * nb + n
            e_, Be_ = dslot.pop(i)
            bi = BvE
            vE_ = vE
            o_, Bo_ = ps_o.next()
            P.op("pe", lambda e: e.matmul(o_[:, 0:128], lhsT=vE_[:, blk, h, :], rhs=e_[:, 0:128], start=True, stop=(n == 0)),
                 reads=[bi, Be_], writes=[Bo_], inc=(n == 0))
            if n > 0:
                P.op("pe", lambda e: e.matmul(o_[:, 0:128], lhsT=vE_[:, blk - 1, h, :], rhs=e_[:, 128:256], start=False, stop=True),
                     reads=[bi, Be_], writes=[Bo_])
            ov = Oacc[:, h, :].rearrange("p (n i dd) -> p n i dd", i=128, dd=d)[:, n, :, r]
            o3 = o_[:, 0:128]
            if d == 1:
                P.op("dve", lambda e: e.tensor_copy(out=ov, in_=o3), reads=[Bo_], writes=[BO[c]])
            else:
                P.op("dve", lambda e: e.tensor_tensor(out=ov, in0=ov, in1=o3, op=ALU.add), reads=[Bo_, BO[c]], writes=[BO[c]])

        def run_pipe(dsteps):
            nsteps = len(dsteps)
            for j in range(nsteps + LA):
                if j < nsteps:
                    dA(j, *dsteps[j])
                if j - LA >= 0:
                    dB(j - LA, *dsteps[j - LA])

        import os
        for d in [int(v) for v in os.environ.get('KDIL', '1,4,16').split(',')]:
            nb = 32 // d
            for r in range(d):
                src = v_d[:, 0:256].rearrange("(n jj dd) c -> dd jj n c", jj=128, dd=d)[r]
                P.dma("sp", vs[:, r * nb:(r + 1) * nb, :], src, writes=[Bvs], semkey="ld1")
            for h in range(4):
                off = 0 if h % 2 == 0 else 64
                if h % 2 == 0:
                    P.op("dve", lambda e, h=h, off=off: e.tensor_copy(out=vE[:, :, h, off:off + 64], in_=vs[:, :, h * 64:(h + 1) * 64]),
                         reads=[Bvs], writes=[BvE])
                else:
                    P.op("act", lambda e, h=h, off=off: e.copy(out=vE[:, :, h, off:off + 64], in_=vs[:, :, h * 64:(h + 1) * 64]),
                         reads=[Bvs], writes=[BvE])
            dsteps = []
            for r in range(d):
                for n in range(nb):
                    for h in range(4):
                        dsteps.append((d, r, n, h, nb))
            run_pipe(dsteps)

        tmp, Btmp = sc.sb("tmpr", [128, 1024], F32), Buf()
        yc, Byc = sc.sb("yc", [128, 2, S], BF16), Buf()
        for h in range(4 if not os.environ.get('KNOFIN') else 0):
            c = h // 2
            own = slice(0, 64) if h % 2 == 0 else slice(64, 128)
            oth = slice(64, 128) if h % 2 == 0 else slice(0, 64)
            for q4 in range(4):
                cs = slice(q4 * 1024, (q4 + 1) * 1024)
                P.op("dve", lambda e, own=own, oth=oth, h=h, cs=cs: e.reciprocal(out=tmp[own, :], in_=Oacc[oth, h, cs]), reads=[BO[c]], writes=[Btmp])
                P.op("dve", lambda e, own=own, h=h, c=c, cs=cs: e.tensor_tensor(out=yc[own, c, cs], in0=Oacc[own, h, cs], in1=tmp[own, :], op=ALU.mult),
                     reads=[BO[c], Btmp], writes=[Byc])
        P.dma("sp", yT_d.rearrange("(c p) t -> p c t", p=128)[:, 4:6, :], yc, reads=[Byc], semkey="st0")
        P.barrier()
        P.flush()
        sc.close()

    def phase_diff(l):
        lam_init = 0.8 - 0.6 * math.exp(-0.3 * l)
        sc = Scope(nc)
        qT, Bq = sc.sb("qT", [128, 2, S], BF16), Buf()
        qkv = qk_d.rearrange("(c p) t -> p c t", p=128)
        P.dma("sp", qT, qkv[:, 6:8, :], writes=[Bq], semkey="ld0")
        kvar = {}
        for hh_ in range(2):
            for m_ in range(2):
                kT_, Bk_ = sc.sb("kdT", [128, 2, S], BF16), Buf()
                s0_ = 8 + 2 * (2 * hh_ + m_)
                P.dma("sp", kT_, qkv[:, s0_:s0_ + 2, :], writes=[Bk_], semkey="ld0")
                kvar[(hh_, m_)] = (kT_, Bk_)
        vs, Bvs = sc.sb("vs", [128, 32, 256], BF16), Buf()
        vE, BvE = sc.sb("vE", [128, 32, 4, 128], BF16), Buf()
        P.op("dve", lambda e: e.memset(vE, 1.0), writes=[BvE])
        P.dma("sp", vs, v_d[:, 256:512].rearrange("(n p) c -> p n c", p=128), writes=[Bvs], semkey="ld1")
        for h in range(4):
            off = 0 if h % 2 == 0 else 64
            if h % 2 == 0:
                P.op("dve", lambda e, h=h, off=off: e.tensor_copy(out=vE[:, :, h, off:off + 64], in_=vs[:, :, h * 64:(h + 1) * 64]),
                     reads=[Bvs], writes=[BvE])
            else:
                P.op("act", lambda e, h=h, off=off: e.copy(out=vE[:, :, h, off:off + 64], in_=vs[:, :, h * 64:(h + 1) * 64]),
                     reads=[Bvs], writes=[BvE])
        cv = sc.sb("cv", [128, 16], F32)
        lamb = sc.sb("lamb", [128, 128], F32)
        lst = sc.sb("lst", [128, 8], F32)
        jl = sc.sb("jl", [128, 32], F32)
        Bl = Buf()
        P.dma("sp", cv, cvec[l], writes=[Bl], semkey="ld2")
        P.dma("sp", lamb, diff_lam[l].partition_broadcast(128), writes=[Bl], semkey="ld2")
        for i in range(2):
            P.op("dve", lambda e, i=i: e.scalar_tensor_tensor(out=jl, in0=lamb[:, 64 * i:64 * i + 32], scalar=1.0,
                                                              in1=lamb[:, 64 * i + 32:64 * i + 64], op0=ALU.mult, op1=ALU.mult,
                                                              accum_out=lst[:, i:i + 1]), reads=[Bl], writes=[Bl])
        P.op("act", lambda e: e.activation(out=lst[:, 2:4], in_=lst[:, 0:2], func=AF.Exp), reads=[Bl], writes=[Bl])
        P.op("dve", lambda e: e.tensor_tensor(out=lst[:, 4:5], in0=lst[:, 3:4], in1=lst[:, 2:3], op=ALU.subtract), reads=[Bl], writes=[Bl])
        P.op("dve", lambda e: e.tensor_scalar(out=lst[:, 4:5], in0=lst[:, 4:5], scalar1=-lam_init, scalar2=None, op0=ALU.add),
             reads=[Bl], writes=[Bl])
        ps_s = Rot([(sc.ps("ps_s", [128, 512]), Buf()) for _ in range(4)])
        ps_o = Rot([(sc.ps("ps_o", [128, 512]), Buf()) for _ in range(3)])
        ps_n, Bpn = sc.ps("ps_n", [128, 512]), Buf()
        ps_junk = ps_n
        NDUM = int(os.environ.get("KDUM", "0"))
        NBURST_ALL = os.environ.get("KBALL", "0") == "1"
        es = Rot([(sc.sb("es", [128, 512], BF16), Buf()) for _ in range(6)])
        rl, Brl = sc.sb("rl", [128, 512], F32), Buf()
        om = Rot([(sc.sb("om", [128, 2, 512], F32), Buf()) for _ in range(2)])
        osb = Rot([(sc.sb("osb", [128, 512], F32), Buf()) for _ in range(2)])
        sq, Bsq = sc.sb("sq", [128, 512], F32), Buf()
        rs, Brs = sc.sb("rs", [128, 512], F32), Buf()
        yst = Rot([(sc.sb("yst", [128, 512], BF16), Buf()) for _ in range(2)])
        scale = 32.0 ** -0.5
        LA = 3
        steps = []
        slot = {}

        def mkA(i, c, qc, hh, m, kb):
            def A():
                rows = slice(0, 128)
                kT, Bk = kvar[(hh, m)]
                off = max(0, 128 * kb - 512 * qc)
                s_, Bs_ = ps_s.next()
                e_, Be_ = es.next()
                slot[i] = (e_, Be_)
                P.op("pe", lambda e: e.matmul(
                    s_[:, off:512], lhsT=kT[rows, c, kb * 128:(kb + 1) * 128], rhs=qT[rows, c, qc * 512 + off:(qc + 1) * 512],
                    start=True, stop=True), reads=[Bk, Bq], writes=[Bs_])
                if kb == 0 and (NBURST_ALL or (hh == 0 and m == 0)):
                    for _ in range(NDUM):
                        P.op("pe", lambda e: e.matmul(ps_junk, lhsT=ones_bf, rhs=mask, start=True, stop=True), inc=False)
                P.op("act", lambda e: e.activation(out=e_[:, off:512], in_=s_[:, off:512], func=AF.Exp, scale=scale),
                     reads=[Bs_], writes=[Be_])
                if kb >= 4 * qc:
                    P.op("pool", lambda e: e.tensor_tensor(out=e_[:, off:off + 128], in0=e_[:, off:off + 128],
                                                           in1=mask[:, 0:128], op=ALU.mult), reads=[Be_, Bc], writes=[Be_])
            return A

        grp = {}

        def mkB(i, c, qc, hh, m, kb):
            def B():
                rows = slice(hh * 64, hh * 64 + 64)
                oth = slice((1 - hh) * 64, (1 - hh) * 64 + 64)
                h = 2 * c + hh
                nkb = 4 * qc + 4
                off = max(0, 128 * kb - 512 * qc)
                if kb == 0:
                    grp["o"] = ps_o.next()
                    if hh == 0 and m == 0:
                        grp["om"] = om.next()
                o_, Bo_ = grp["o"]
                om_, Bom = grp["om"]
                e_, Be_ = slot.pop(i)
                P.op("pe", lambda e: e.matmul(o_[:, off:512], lhsT=vE[:, kb, h, :], rhs=e_[:, off:512],
                                              start=(kb == 0), stop=(kb == nkb - 1)),
                     reads=[BvE, Be_], writes=[Bo_], inc=(kb == nkb - 1))
                if kb < nkb - 1:
                    return
                P.op("dve", lambda e: e.reciprocal(out=rl[rows, :], in_=o_[oth, :]), reads=[Bo_], writes=[Brl])
                P.op("dve", lambda e: e.tensor_tensor(out=om_[rows, m, :], in0=o_[rows, :], in1=rl[rows, :], op=ALU.mult),
                     reads=[Bo_, Brl], writes=[Bom])
                if not (hh == 1 and m == 1):
                    return
                ob, Bob = osb.next()
                P.op("dve", lambda e: e.scalar_tensor_tensor(out=ob, in0=om_[:, 1, :], scalar=lst[:, 4:5], in1=om_[:, 0, :],
                                                             op0=ALU.mult, op1=ALU.add), reads=[Bom, Bl], writes=[Bob])
                P.op("act", lambda e: e.activation(out=sq, in_=ob, func=AF.Square), reads=[Bob], writes=[Bsq])
                P.op("pe", lambda e: e.matmul(ps_n, lhsT=blockones, rhs=sq, start=True, stop=True), reads=[Bc, Bsq], writes=[Bpn])
                P.op("dve", lambda e: e.tensor_scalar(out=rs, in0=ps_n, scalar1=1.0 / 64, scalar2=EPS, op0=ALU.mult, op1=ALU.add),
                     reads=[Bpn], writes=[Brs])
                P.op("act", lambda e: e.activation(out=rs, in_=rs, func=AF.Sqrt), reads=[Brs], writes=[Brs])
                P.op("dve", lambda e: e.reciprocal(out=rs, in_=rs), reads=[Brs], writes=[Brs])
                P.op("dve", lambda e: e.tensor_tensor(out=ob, in0=ob, in1=rs, op=ALU.mult), reads=[Bob, Brs], writes=[Bob])
                ys, Bys = yst.next()
                P.op("dve", lambda e: e.tensor_scalar(out=ys, in0=ob, scalar1=cv[:, 8:9], scalar2=(1.0 - lam_init),
                                                      op0=ALU.mult, op1=ALU.mult), reads=[Bob, Bl], writes=[Bys])
                P.dma("sp", yT_d[(6 + c) * 128:(7 + c) * 128, qc * 512:(qc + 1) * 512], ys, reads=[Bys])
            return B

        i = 0
        for c in range(2):
            for qc in range(8):
                for hh in range(2):
                    for m in range(2):
                        for kb in range(4 * qc + 4):
                            steps.append((mkA(i, c, qc, hh, m, kb), mkB(i, c, qc, hh, m, kb)))
                            i += 1
        n = len(steps)
        for j in range(n + LA):
            if j < n:
                steps[j][0]()
            if j - LA >= 0:
                steps[j - LA][1]()
        P.barrier()
        P.flush()
        sc.close()

    def phase_ffn(l, x_src):
        moe = (l % 2 == 1)
        last = (l == 1)
        osc = Scope(nc)
        yacc = osc.sb("yacc", [128, 16, D], F32)
        By = bufs(16)
        h2T = osc.sb("h2T", [128, 8, 2048], BF16)
        Bh = bufs(4)
        gates = osc.sb("gates", [128, 16, 8], F32)
        Bgt = bufs(16)
        wo = osc.sb("wo", [128, 8, D], BF16)
        Bwo = Buf()
        for k in range(8):
            P.dma("pool", wo[:, k, :], w_out[l, k * 128:(k + 1) * 128, :], writes=[Bwo], semkey="w")
        for ps_ in range(2):
            sc = Scope(nc)
            g2 = sc.sb("g2", [128, D], F32)
            Bg = Buf()
            P.dma("sp", g2, norm2_g[l:l + 1, :].partition_broadcast(128), writes=[Bg], semkey="c")
            if moe:
                rB = sc.sb("rB", [128, NE, D], F32)
                P.dma("sp", rB, routerT.partition_broadcast(128).rearrange("p o (e d) -> p (o e) d", e=NE), writes=[Bg], semkey="c")
                h2f = Rot([(sc.sb("h2f", [128, D], F32), Buf()) for _ in range(2)])
                jf, Bjf = sc.sb("jf", [128, D], F32), Buf()
                lg = sc.sb("lg", [128, 16, 8], F32)
                sm = sc.sb("sm", [128, 16, 32], F32)
                Blg = bufs(16)
            xs = Rot([(sc.sb("xs", [128, D], F32), Buf()) for _ in range(4)])
            yTs = Rot([(sc.sb("yTs", [128, 8, 128], BF16), Buf()) for _ in range(4)])
            hbf = Rot([(sc.sb("hbf", [128, D], BF16), Buf()) for _ in range(3)])
            junk, Bj = sc.sb("junk", [128, D], BF16), Buf()
            stat = sc.sb("stat", [128, 16, 2], F32)
            Bst = bufs(16)
            po = Rot([(sc.ps("po", [128, D]), Buf()) for _ in range(3)])
            pT = Rot([(sc.ps("pT", [128, 8, 128], BF16), Buf()) for _ in range(2)])
            wres = {}
            nres = {}

            def w_stage(tt):
                gt = ps_ * 16 + tt
                xt, Bx = xs.next()
                yt, Byt = yTs.next()
                P.dma("sp", xt, x_src[gt * 128:(gt + 1) * 128, :], writes=[Bx], semkey=f"x{tt % 2}")
                P.dma("sp", yt, yT_d.rearrange("(k p) t -> p k t", p=128)[:, :, gt * 128:(gt + 1) * 128], writes=[Byt], semkey=f"x{tt % 2}")
                p_, Bp_ = po.next()
                for half in range(2):
                    for k in range(8):
                        P.op("pe", lambda e, p_=p_, half=half, k=k, yt=yt: e.matmul(
                            p_[:, half * 512:(half + 1) * 512], lhsT=yt[:, k, :], rhs=wo[:, k, half * 512:(half + 1) * 512],
                            start=(k == 0), stop=(k == 7)), reads=[Byt, Bwo], writes=[Bp_], inc=(half == 1 and k == 7))
                wres[tt] = (xt, Bx, p_, Bp_)

            def n_stage(tt):
                xt, Bx, p_, Bp_ = wres.pop(tt)
                ya = yacc[:, tt, :]
                P.op("dve", lambda e, ya=ya, p_=p_, xt=xt: e.tensor_tensor(out=ya, in0=p_, in1=xt, op=ALU.add), reads=[Bp_, Bx], writes=[By[tt]])
                st = stat[:, tt, :]
                rstd_ops(st, Bst[tt], 1.0 / D, ya, By[tt], junk, Bj)
                hb, Bhb = hbf.next()
                if not moe:
                    P.op("dve", lambda e, hb=hb, ya=ya, st=st: e.scalar_tensor_tensor(out=hb, in0=ya, scalar=st[:, 1:2], in1=g2,
                                                                                     op0=ALU.mult, op1=ALU.mult),
                         reads=[By[tt], Bst[tt], Bg], writes=[Bhb])
                else:
                    hf, Bhf = h2f.next()
                    P.op("dve", lambda e, hf=hf, ya=ya, st=st: e.scalar_tensor_tensor(out=hf, in0=ya, scalar=st[:, 1:2], in1=g2,
                                                                                     op0=ALU.mult, op1=ALU.mult),
                         reads=[By[tt], Bst[tt], Bg], writes=[Bhf])
                    P.op("act", lambda e, hb=hb, hf=hf: e.copy(out=hb, in_=hf), reads=[Bhf], writes=[Bhb])
                    L = lg[:, tt, :]
                    BL = Blg[tt]
                    for ex in range(NE):
                        P.op("dve", lambda e, hf=hf, ex=ex, L=L: e.scalar_tensor_tensor(
                            out=jf, in0=hf, scalar=1.0, in1=rB[:, ex, :], op0=ALU.mult, op1=ALU.mult, accum_out=L[:, ex:ex + 1]),
                            reads=[Bhf, Bg], writes=[Bjf, BL])
                    m = sm[:, tt, :]
                    P.op("dve", lambda e, m=m, L=L: e.tensor_reduce(out=m[:, 0:1], in_=L, axis=AX.X, op=ALU.max), reads=[BL], writes=[BL])
                    P.op("dve", lambda e, m=m, L=L: e.tensor_scalar(out=m[:, 8:16], in0=L, scalar1=m[:, 0:1], scalar2=None, op0=ALU.is_equal),
                         reads=[BL], writes=[BL])
                    P.op("dve", lambda e, m=m, L=L: e.scalar_tensor_tensor(out=m[:, 16:24], in0=m[:, 8:16], scalar=-1e30, in1=L,
                                                                          op0=ALU.mult, op1=ALU.add), reads=[BL], writes=[BL])
                    P.op("dve", lambda e, m=m: e.tensor_reduce(out=m[:, 1:2], in_=m[:, 16:24], axis=AX.X, op=ALU.max), reads=[BL], writes=[BL])
                    P.op("dve", lambda e, m=m: e.tensor_scalar(out=m[:, 16:24], in0=m[:, 16:24], scalar1=m[:, 1:2], scalar2=None, op0=ALU.is_equal),
                         reads=[BL], writes=[BL])
                    P.op("dve", lambda e, m=m: e.tensor_tensor(out=m[:, 2:3], in0=m[:, 1:2], in1=m[:, 0:1], op=ALU.subtract), reads=[BL], writes=[BL])
                    P.op("act", lambda e, m=m: e.activation(out=m[:, 2:3], in_=m[:, 2:3], func=AF.Exp), reads=[BL], writes=[BL])
                    P.op("dve", lambda e, m=m: e.tensor_scalar(out=m[:, 3:4], in0=m[:, 2:3], scalar1=1.0, scalar2=None, op0=ALU.add), reads=[BL], writes=[BL])
                    P.op("dve", lambda e, m=m: e.reciprocal(out=m[:, 3:4], in_=m[:, 3:4]), reads=[BL], writes=[BL])
                    P.op("dve", lambda e, m=m: e.tensor_tensor(out=m[:, 4:5], in0=m[:, 2:3], in1=m[:, 3:4], op=ALU.mult), reads=[BL], writes=[BL])
                    P.op("dve", lambda e, m=m: e.tensor_scalar(out=m[:, 24:32], in0=m[:, 8:16], scalar1=m[:, 3:4], scalar2=None, op0=ALU.mult),
                         reads=[BL], writes=[BL])
                    P.op("dve", lambda e, m=m, tt=tt: e.scalar_tensor_tensor(out=gates[:, tt, :], in0=m[:, 16:24], scalar=m[:, 4:5], in1=m[:, 24:32],
                                                                            op0=ALU.mult, op1=ALU.add), reads=[BL], writes=[Bgt[tt]])
                nres[tt] = (hb, Bhb)

            def t_stage(tt):
                hb, Bhb = nres.pop(tt)
                pt_, Bpt = pT.next()
                for k in range(8):
                    P.op("pe", lambda e, pt_=pt_, hb=hb, k=k: e.transpose(pt_[:, k, :], hb[:, k * 128:(k + 1) * 128], ident),
                         reads=[Bhb, Bc], writes=[Bpt], inc=(k == 7))
                P.op("act", lambda e, pt_=pt_, tt=tt: e.copy(out=h2T[:, :, tt * 128:(tt + 1) * 128], in_=pt_), reads=[Bpt], writes=[Bh[tt // 4]])

            LA3 = 2
            for tt in range(min(LA3, 16)):
                w_stage(tt)
            for tt in range(16):
                if tt + LA3 < 16:
                    w_stage(tt + LA3)
                n_stage(tt)
                t_stage(tt)
            P.barrier()
            P.flush()
            sc.close()
            sc = Scope(nc)
            wg = [(sc.sb("wg", [128, 8, 512], BF16), Buf()) for _ in range(2)]
            wu = [(sc.sb("wu", [128, 8, 512], BF16), Buf()) for _ in range(2)]
            wd = [(sc.sb("wd", [128, 4, D], BF16), Buf()) for _ in range(2)]
            act = Rot([(sc.sb("act", [128, 4, 512], BF16), Buf()) for _ in range(3)])
            sg = Rot([(sc.sb("sg", [128, 512], F32), Buf()) for _ in range(3)])
            pg = Rot([(sc.ps("pg", [128, 512]), Buf()) for _ in range(2)])
            pu = Rot([(sc.ps("pu", [128, 512]), Buf()) for _ in range(2)])
            pd = Rot([(sc.ps("pd", [128, D]), Buf()) for _ in range(2)])
            slabs = []
            if not moe:
                f0 = 0
                while f0 < DFF:
                    fw = min(512, DFF - f0)
                    slabs.append((ffn_wg[0], ffn_wu[0], ffn_wd[0], None, f0, fw))
                    f0 += fw
            else:
                for ex in range(NE):
                    for f0 in range(0, DFE, 512):
                        slabs.append((moe_wg[0, ex], moe_wu[0, ex], moe_wd[0, ex], ex, f0, 512))

            def load(i):
                g_, u_, d_, ex, f0, fw = slabs[i]
                b = i % 2
                P.dma("pool", wg[b][0][:, :, 0:fw], g_[:, f0:f0 + fw].rearrange("(k p) f -> p k f", p=128), writes=[wg[b][1]], semkey=f"wg{b}")
                P.dma("pool", wu[b][0][:, :, 0:fw], u_[:, f0:f0 + fw].rearrange("(k p) f -> p k f", p=128), writes=[wu[b][1]], semkey=f"wu{b}")
                P.dma("pool", wd[b][0][:, 0:fw // 128, :], d_[f0:f0 + fw, :].rearrange("(k p) n -> p k n", p=128), writes=[wd[b][1]], semkey=f"wd{b}")

            acts = {}

            def gu_stage(i, cq):
                g_, u_, d_, ex, f0, fw = slabs[i]
                b = i % 2
                nfc = fw // 128
                wgt, Bwg = wg[b]
                wut, Bwu = wu[b]
                a_, Ba_ = act.next()
                acts[(i, cq)] = (a_, Ba_)
                for fc in range(nfc):
                    pg_, Bpg = pg.next()
                    pu_, Bpu = pu.next()
                    for (pz, Bpz, wz, Bwz) in ((pg_, Bpg, wgt, Bwg), (pu_, Bpu, wut, Bwu)):
                        for k in range(8):
                            P.op("pe", lambda e, pz=pz, wz=wz, k=k, fc=fc, cq=cq: e.matmul(
                                pz, lhsT=wz[:, k, fc * 128:(fc + 1) * 128], rhs=h2T[:, k, cq * 512:(cq + 1) * 512],
                                start=(k == 0), stop=(k == 7)), reads=[Bwz, Bh[cq]], writes=[Bpz], inc=(k == 7))
                    s_, Bs_ = sg.next()
                    P.op("act", lambda e, s_=s_, pg_=pg_: e.activation(out=s_, in_=pg_, func=AF.Silu), reads=[Bpg], writes=[Bs_])
                    P.op("dve", lambda e, a_=a_, fc=fc, pu_=pu_, s_=s_: e.tensor_tensor(out=a_[:, fc, :], in0=pu_, in1=s_, op=ALU.mult),
                         reads=[Bpu, Bs_], writes=[Ba_])

            def d_stage(i, cq):
                g_, u_, d_, ex, f0, fw = slabs[i]
                b = i % 2
                nfc = fw // 128
                wdt, Bwd = wd[b]
                a_, Ba_ = acts.pop((i, cq))
                for t in range(4):
                    tt = cq * 4 + t
                    pd_, Bpd = pd.next()
                    for half in range(2):
                        for fc in range(nfc):
                            P.op("pe", lambda e, pd_=pd_, half=half, fc=fc, a_=a_, t=t, wdt=wdt, nfc=nfc: e.matmul(
                                pd_[:, half * 512:(half + 1) * 512], lhsT=a_[:, fc, t * 128:(t + 1) * 128],
                                rhs=wdt[:, fc, half * 512:(half + 1) * 512], start=(fc == 0), stop=(fc == nfc - 1)),
                                reads=[Ba_, Bwd], writes=[Bpd], inc=(half == 1 and fc == nfc - 1))
                    ya = yacc[:, tt, :]
                    if ex is None:
                        P.op("dve", lambda e, ya=ya, pd_=pd_: e.tensor_tensor(out=ya, in0=pd_, in1=ya, op=ALU.add), reads=[Bpd, By[tt]], writes=[By[tt]])
                    else:
                        P.op("dve", lambda e, ya=ya, pd_=pd_, tt=tt, ex=ex: e.scalar_tensor_tensor(
                            out=ya, in0=pd_, scalar=gates[:, tt, ex:ex + 1], in1=ya, op0=ALU.mult, op1=ALU.add),
                            reads=[Bpd, By[tt], Bgt[tt]], writes=[By[tt]])

            units = [(i, cq) for i in range(len(slabs)) for cq in range(4)]
            load(0)
            if len(slabs) > 1:
                load(1)
            gu_stage(*units[0])
            for u in range(len(units)):
                if u + 1 < len(units):
                    gu_stage(*units[u + 1])
                d_stage(*units[u])
                i, cq = units[u]
                if cq == 3 and i + 2 < len(slabs):
                    load(i + 2)
            P.barrier()
            P.flush()
            sc.close()
            sc = Scope(nc)
            if not last:
                for q4 in range(4):
                    P.dma("sp", xmid.rearrange("(n p) d -> p n d", p=128)[:, ps_ * 16 + q4 * 4: ps_ * 16 + q4 * 4 + 4, :],
                          yacc[:, q4 * 4:(q4 + 1) * 4, :], reads=By[q4 * 4:(q4 + 1) * 4], semkey="xo")
            else:
                gf = sc.sb("gf", [128, D], F32)
                Bgf = Buf()
                P.dma("sp", gf, final_g.partition_broadcast(128), writes=[Bgf], semkey="c")
                junk, Bj = sc.sb("junk", [128, D], BF16), Buf()
                stat = sc.sb("stat", [128, 16, 2], F32)
                Bst = bufs(16)
                ost = Rot([(sc.sb("ost", [128, D], F32), Buf()) for _ in range(3)])
                for tt in range(16):
                    ya = yacc[:, tt, :]
                    st = stat[:, tt, :]
                    rstd_ops(st, Bst[tt], 1.0 / D, ya, By[tt], junk, Bj)
                    o_, Bo_ = ost.next()
                    P.op("dve", lambda e, o_=o_, ya=ya, st=st: e.scalar_tensor_tensor(out=o_, in0=ya, scalar=st[:, 1:2], in1=gf,
                                                                                     op0=ALU.mult, op1=ALU.mult),
                         reads=[By[tt], Bst[tt], Bgf], writes=[Bo_])
                    gt = ps_ * 16 + tt
                    P.dma("sp", out[gt * 128:(gt + 1) * 128, :], o_, reads=[Bo_], semkey=f"xo{tt % 3}")
            P.barrier()
            P.flush()
            sc.close()
        osc.close()

    for l in range(nlayers):
        x_src = x_in if l == 0 else xmid
        for nm, fn in (("p1", lambda: phase_p1(l, x_src)), ("conv", lambda: phase_conv(l)), ("pool", lambda: phase_pool(l)),
                       ("dil", lambda: phase_dil(l)), ("diff", lambda: phase_diff(l)), ("ffn", lambda: phase_ffn(l, x_src))):
            if only is None or nm in only:
                fn()
    P.barrier()
    P.flush()
    gs.close()
    return nc, P


def _rope_np(seq, rot):
    pos = np.arange(seq, dtype=np.float32)
    inv = (np.float32(THETA) ** (-(np.arange(0, rot, 2, dtype=np.float32)) / np.float32(rot))).astype(np.float32)
    ang = (pos[:, None] * inv[None, :]).astype(np.float32)
    return np.cos(ang).astype(np.float32), np.sin(ang).astype(np.float32)


def _consts():
    cos_c, sin_c = _rope_np(S, 16)
    cos_d, sin_d = _rope_np(S, 8)
    rope = np.zeros((4, 128, S), np.float32)
    for p in range(128):
        i = p % 64
        if i < 8:
            rope[0, p] = cos_c[:, i]
            rope[1, p] = -sin_c[:, i]
        elif i < 16:
            rope[0, p] = cos_c[:, i - 8]
            rope[1, p] = sin_c[:, i - 8]
        else:
            rope[0, p] = 1.0
        i = p % 32
        if i < 4:
            rope[2, p] = cos_d[:, i]
            rope[3, p] = -sin_d[:, i]
        elif i < 8:
            rope[2, p] = cos_d[:, i - 4]
            rope[3, p] = sin_d[:, i - 4]
        else:
            rope[2, p] = 1.0
    cst = np.zeros((128, 928), np.float32)
    cst[:, 0:128] = np.eye(128, dtype=np.float32)
    cst[0:64, 128:192] = 1.0
    cst[64:128, 192:256] = 1.0
    cst[:, 256:384] = 1.0
    jj = np.arange(128)[:, None]
    ii = np.arange(128)[None, :]
    m = np.concatenate([(jj <= ii), (jj >= ii)], axis=1).astype(np.float32)
    cst[:, 384:640] = m
    cst[:, 640:896] = m
    t = np.arange(16, dtype=np.float32)
    for c in range(2):
        for p in range(128):
            w = (2, 4, 8, 16)[c * 2 + p // 64]
            cst[p, 896 + c * 16: 896 + (c + 1) * 16] = 1.0 / np.minimum(t + 1.0, float(w))
    return rope, cst


def _perm_cols():
    idx = []
    for base, hd, half in ((3 * G, 64, 8), (4 * G, 64, 8), (6 * G, 32, 4), (7 * G, 32, 4)):
        for j in range(G):
            i = j % hd
            if i < half:
                pj = j + half
            elif i < 2 * half:
                pj = j - half
            else:
                pj = j
            idx.append(base + pj)
    return np.asarray(idx)


_CACHE = {}


def kernel(x, norm1_g, w_in, conv_dw_w, conv_dw_b, conv_ln_g, conv_ln_b, conv_pw_w,
           pool_w, pool_scale, diff_lam, diff_ln_g, w_out, norm2_g,
           ffn_w_gate, ffn_w_up, ffn_w_down, moe_router, moe_w_gate, moe_w_up, moe_w_down,
           final_g):
    f = lambda a: np.ascontiguousarray(np.asarray(a, dtype=np.float32))
    if "nc" not in _CACHE:
        _CACHE["nc"] = build_program()[0]
        _CACHE["consts"] = _consts()
    nc = _CACHE["nc"]
    rope, cst = _CACHE["consts"]
    w_in = f(w_in)
    w_inP = np.ascontiguousarray(w_in[:, :, _perm_cols()])
    col = lambda v: np.asarray(v, np.float32).reshape(2, 2, 128).transpose(0, 2, 1)
    cvec = np.zeros((2, 128, 16), np.float32)
    cvec[:, :, 0:2] = col(conv_dw_b)
    cvec[:, :, 2:4] = col(conv_ln_g)
    cvec[:, :, 4:6] = col(conv_ln_b)
    cvec[:, :, 6:8] = col(pool_scale)
    cvec[:, :, 8] = np.concatenate([np.asarray(diff_ln_g, np.float32)] * 2, axis=1)
    pp = np.arange(128)
    for j in range(4):
        cvec[:, :, 9 + j] = ((pp // 32) == j).astype(np.float32)
    for j in range(2):
        cvec[:, :, 13 + j] = ((pp // 64) == j).astype(np.float32)
    dwcol = np.ascontiguousarray(np.asarray(conv_dw_w, np.float32).reshape(2, 31, 2, 128).transpose(0, 3, 2, 1)).reshape(2, 128, 62)
    shared = {
        "norm1_g": f(norm1_g), "w_in": w_in, "w_inP": w_inP, "dwcol": dwcol, "cvec": cvec,
        "conv_pw_w": f(conv_pw_w), "pool_w": f(pool_w), "diff_lam": f(diff_lam).reshape(2, 1, 128),
        "w_out": f(w_out), "norm2_g": f(norm2_g), "ffn_w_gate": f(ffn_w_gate), "ffn_w_up": f(ffn_w_up),
        "ffn_w_down": f(ffn_w_down), "routerT": np.ascontiguousarray(f(moe_router)[0].T).reshape(1, NE * D),
        "moe_w_gate": f(moe_w_gate), "moe_w_up": f(moe_w_up), "moe_w_down": f(moe_w_down),
        "final_g": f(final_g).reshape(1, D), "ropeT": rope, "cst": cst,
    }
    x = f(x)
    in_maps = [dict(shared, x=x[b]) for b in range(8)]
    res = run_bass_kernel_spmd(nc, in_maps, core_ids=list(range(8)))
    return np.stack([np.asarray(r["out"], dtype=np.float32) for r in res.results], axis=0)
```

```python
import math
from contextlib import ExitStack
import numpy as np
import concourse.bass as bass
import concourse.mybir as mybir
from concourse.bass_utils import run_bass_kernel_spmd

F32 = mybir.dt.float32
BF16 = mybir.dt.bfloat16
AF = mybir.ActivationFunctionType
ALU = mybir.AluOpType
AX = mybir.AxisListType

S = 4096
D = 1024
G = 256
NT = 32
DFF = 2816
DFE = 3584
NE = 8
EPS = 1e-5
THETA = 500000.0


import os
ATTACH_WAITS = os.environ.get('KATTACH', '1') == '1'


class Buf:
    __slots__ = ("w", "r")

    def __init__(self):
        self.w = None
        self.r = {}


def bufs(n):
    return [Buf() for _ in range(n)]


class Eng:
    def __init__(self, name, sem):
        self.name = name
        self.sem = sem
        self.count = 0
        self.ops = []
        self.waited = {}
        self.pend_r = []
        self.pend_w = []


class Prog:
    def __init__(self, nc):
        self.nc = nc
        self.sems = {}
        self.semcount = {}
        self.E = {}
        for n in ("pe", "act", "dve", "pool", "sp"):
            h = nc.alloc_semaphore("s_" + n)
            self.sems[n] = h
            self.E[n] = Eng(n, h)
        self.nops = 0
        self.free_dma = {}
        self.gen = 0
        self.all_dma = []
        self.keep = []

    def _wait(self, e, key, val, acc=None):
        if e.name == "pe" and key == "pe":
            return
        if key not in self.sems:
            return
        if e.waited.get(key, 0) >= val:
            return
        e.waited[key] = val
        h = self.sems[key]
        if acc is not None:
            acc.append((h, val))
        else:
            e.ops.append(lambda eh, h=h, val=val: eh.wait_ge(h, val))

    def _deps(self, e, reads, writes, acc=None):
        for b in reads:
            if b.w is not None:
                self._wait(e, *b.w, acc=acc)
        for b in writes:
            if b.w is not None:
                self._wait(e, *b.w, acc=acc)
            for k, v in b.r.items():
                self._wait(e, k, v, acc=acc)

    def op(self, eng, fn, reads=(), writes=(), inc=True):
        e = self.E[eng]
        acc = [] if ATTACH_WAITS else None
        self._deps(e, reads, writes, acc)
        att = None
        if acc:
            att = acc.pop()
            for (h, val) in acc:
                e.ops.append(lambda eh, h=h, val=val: eh.wait_ge(h, val))
        e.pend_r.extend(reads)
        e.pend_w.extend(writes)
        self.nops += 1

        def emit(eh, fn=fn, att=att):
            ins = fn(eh)
            if att is not None:
                ins = ins._wait_ge(att[0], att[1])
            return ins
        if inc:
            e.count += 1
            ev = (eng, e.count)
            h = e.sem
            e.ops.append(lambda eh, emit=emit, h=h: emit(eh).then_inc(h, 1))
            for b in e.pend_w:
                b.w = ev
                b.r = {}
            for b in e.pend_r:
                if b.w != ev:
                    b.r[eng] = e.count
            e.pend_r = []
            e.pend_w = []
        else:
            e.ops.append(emit)

    def dma(self, queue, out, in_, reads=(), writes=(), semkey=None):
        e = self.E[queue]
        self._deps(e, reads, writes)
        b0 = writes[0] if len(writes) else reads[0]
        semkey = (id(b0), queue, self.gen)
        if semkey not in self.sems:
            if self.free_dma.get(queue):
                h, c = self.free_dma[queue].pop()
            else:
                h, c = self.nc.alloc_semaphore(f"d{len(self.all_dma)}"), 0
                self.all_dma.append(h)
            self.sems[semkey] = h
            self.semcount[semkey] = c
            self.keep.append(b0)
        self.semcount[semkey] += 16
        val = self.semcount[semkey]
        h = self.sems[semkey]
        e.ops.append(lambda eh, out=out, in_=in_, h=h: eh.dma_start(out=out, in_=in_).then_inc(h, 16))
        for b in writes:
            b.w = (semkey, val)
            b.r = {}
        for b in reads:
            b.r[semkey] = val
        self.nops += 1

    def barrier(self):
        snap = dict(self.semcount)
        for n, e in self.E.items():
            assert not e.pend_r and not e.pend_w, n
            snap[n] = e.count
        for n, e in self.E.items():
            for k, v in snap.items():
                if v > 0 and k != n:
                    self._wait(e, k, v)
        self.gen += 1
        for k in list(self.semcount.keys()):
            self.free_dma.setdefault(k[1], []).append((self.sems[k], self.semcount[k]))
            del self.sems[k]
            del self.semcount[k]
            for e in self.E.values():
                e.waited.pop(k, None)

    def flush(self):
        nc = self.nc
        E = self.E
        with nc.Block() as block:
            @block.sync
            def _(eh):
                for f in E["sp"].ops:
                    f(eh)

            @block.tensor
            def _(eh):
                for f in E["pe"].ops:
                    f(eh)

            @block.scalar
            def _(eh):
                for f in E["act"].ops:
                    f(eh)

            @block.vector
            def _(eh):
                for f in E["dve"].ops:
                    f(eh)

            @block.gpsimd
            def _(eh):
                for f in E["pool"].ops:
                    f(eh)
        for e in E.values():
            e.ops = []


class Scope:
    cnt = [0]

    def __init__(self, nc):
        self.nc = nc
        self.st = ExitStack()

    def sb(self, name, shape, dt):
        Scope.cnt[0] += 1
        return self.st.enter_context(self.nc.sbuf_tensor(f"{name}_{Scope.cnt[0]}", list(shape), dt)).ap()

    def ps(self, name, shape, dt=F32):
        Scope.cnt[0] += 1
        return self.st.enter_context(self.nc.psum_tensor(f"{name}_{Scope.cnt[0]}", list(shape), dt)).ap()

    def close(self):
        self.st.close()


class Rot:
    def __init__(self, items):
        self.items = items
        self.i = 0

    def next(self):
        it = self.items[self.i % len(self.items)]
        self.i += 1
        return it


def build_program(debug=False, only=None, nlayers=2):
    nc = bass.Bass("TRN2", target_bir_lowering=False)
    P = Prog(nc)

    def din(name, shape, dt=F32):
        return nc.dram_tensor(name, list(shape), dt, kind="ExternalInput").ap()

    def dscr(name, shape, dt):
        return nc.dram_tensor(name, list(shape), dt, kind=("ExternalOutput" if debug else "Internal")).ap()

    x_in = din("x", [S, D])
    norm1_g = din("norm1_g", [2, D])
    w_in = din("w_in", [2, D, 9 * G])
    w_inP = din("w_inP", [2, D, 4 * G])
    dwcol = din("dwcol", [2, 128, 2 * 31])
    cvec = din("cvec", [2, 128, 16])
    conv_pw_w = din("conv_pw_w", [2, G, G])
    pool_w = din("pool_w", [2, 4, 64, 64])
    diff_lam = din("diff_lam", [2, 1, 128])
    w_out = din("w_out", [2, D, D])
    norm2_g = din("norm2_g", [2, D])
    ffn_wg = din("ffn_w_gate", [1, D, DFF])
    ffn_wu = din("ffn_w_up", [1, D, DFF])
    ffn_wd = din("ffn_w_down", [1, DFF, D])
    routerT = din("routerT", [1, NE * D])
    moe_wg = din("moe_w_gate", [1, NE, D, DFE])
    moe_wu = din("moe_w_up", [1, NE, D, DFE])
    moe_wd = din("moe_w_down", [1, NE, DFE, D])
    final_g = din("final_g", [1, D])
    ropeT = din("ropeT", [4, 128, S])
    cst = din("cst", [128, 128 * 3 + 512 + 32])
    out = nc.dram_tensor("out", [S, D], F32, kind="ExternalOutput").ap()

    zA_d = dscr("zA_d", [2 * 128, S], BF16)
    uB_d = dscr("uB_d", [2 * 128, S], F32)
    qk_d = dscr("qk_d", [16 * 128, S], BF16)
    v_d = dscr("v_d", [S, 512], BF16)
    yT_d = dscr("yT_d", [D, S], BF16)
    xmid = dscr("xmid", [S, D], F32)

    gs = Scope(nc)
    ident = gs.sb("ident", [128, 128], BF16)
    ones_bf = gs.sb("ones_bf", [128, 128], BF16)
    blockones = gs.sb("blockones", [128, 128], F32)
    ones_f = gs.sb("ones_f", [128, 128], F32)
    mask = gs.sb("mask", [128, 512], BF16)
    pooltab = gs.sb("pooltab", [128, 2, 16], F32)
    Bc = Buf()
    P.dma("pool", ident, cst[:, 0:128], writes=[Bc], semkey="c")
    P.dma("pool", blockones, cst[:, 128:256], writes=[Bc], semkey="c")
    P.dma("pool", ones_f, cst[:, 256:384], writes=[Bc], semkey="c")
    P.dma("pool", ones_bf, cst[:, 256:384], writes=[Bc], semkey="c")
    P.dma("pool", mask, cst[:, 384:896], writes=[Bc], semkey="c")
    P.dma("pool", pooltab, cst[:, 896:928].rearrange("p (c t) -> p c t", c=2), writes=[Bc], semkey="c")

    def rstd_ops(stat, Bst, n_inv, src, Bsrc, junk, Bjunk):
        P.op("act", lambda e: e.activation(out=junk, in_=src, func=AF.Square, accum_out=stat[:, 0:1]),
             reads=[Bsrc], writes=[Bjunk, Bst])
        P.op("dve", lambda e: e.tensor_scalar(out=stat[:, 1:2], in0=stat[:, 0:1], scalar1=n_inv, scalar2=EPS,
                                              op0=ALU.mult, op1=ALU.add), reads=[Bst], writes=[Bst])
        P.op("act", lambda e: e.activation(out=stat[:, 1:2], in_=stat[:, 1:2], func=AF.Sqrt), reads=[Bst], writes=[Bst])
        P.op("dve", lambda e: e.reciprocal(out=stat[:, 1:2], in_=stat[:, 1:2]), reads=[Bst], writes=[Bst])

    def phase_p1(l, x_src):
        sc = Scope(nc)
        w_sb = sc.sb("w_in", [128, 8, 3328], BF16)
        Bw = Buf()
        for k in range(8):
            P.dma("pool", w_sb[:, k, 0:2304], w_in[l, k * 128:(k + 1) * 128, :], writes=[Bw], semkey="w")
            P.dma("pool", w_sb[:, k, 2304:3328], w_inP[l, k * 128:(k + 1) * 128, :], writes=[Bw], semkey="w")
        g1 = sc.sb("g1", [128, D], F32)
        cv = sc.sb("cv", [128, 16], F32)
        Bg = Buf()
        P.dma("sp", g1, norm1_g[l:l + 1, :].partition_broadcast(128), writes=[Bg], semkey="c")
        P.dma("sp", cv, cvec[l], writes=[Bg], semkey="c")
        xs = [(sc.sb("xs", [128, 4, D], F32), Buf()) for _ in range(2)]
        rp = [(sc.sb("rope", [128, 4, 512], F32), Buf()) for _ in range(2)]
        hT = [(sc.sb("hT", [128, 8, 512], BF16), Buf()) for _ in range(2)]
        zst = [(sc.sb("zst", [128, 2, 512], BF16), Buf()) for _ in range(2)]
        ust = [(sc.sb("ust", [128, 2, 512], F32), Buf()) for _ in range(2)]
        qst = [(sc.sb("qst", [128, 16, 512], BF16), Buf()) for _ in range(2)]
        vst = [(sc.sb("vst", [128, 4, 512], BF16), Buf()) for _ in range(2)]
        hbf = Rot([(sc.sb("hbf", [128, D], BF16), Buf()) for _ in range(5)])
        tmpf = Rot([(sc.sb("tmpf", [128, 512], F32), Buf()) for _ in range(6)])
        junk = sc.sb("junk", [128, D], BF16)
        Bj = Buf()
        stat = sc.sb("stat", [128, 8, 2], F32)
        Bst = bufs(8)
        pT = Rot([(sc.ps("pT", [128, 8, 128], BF16), Buf()) for _ in range(2)])
        pp = Rot([(sc.ps("pp", [128, 512], F32), Buf()) for _ in range(6)])

        def proj(col0, hTt, BhT):
            p, Bp = pp.next()
            for k in range(8):
                P.op("pe", lambda e, p=p, k=k: e.matmul(p, lhsT=w_sb[:, k, col0:col0 + 128], rhs=hTt[:, k, :],
                                                         start=(k == 0), stop=(k == 7)),
                     reads=[Bw, BhT], writes=[Bp], inc=(k == 7))
            return p, Bp

        hbs = {}

        def n_stage(ci):
            pb = ci % 2
            xt, Bx = xs[pb]
            rt, Br = rp[pb]
            P.dma("sp", xt, x_src[ci * 512:(ci + 1) * 512, :].rearrange("(t p) d -> p t d", p=128), writes=[Bx])
            P.dma("sp", rt, ropeT[:, :, ci * 512:(ci + 1) * 512].rearrange("f p t -> p f t"), writes=[Br])
            for t in range(4):
                si = (ci * 4 + t) % 8
                st = stat[:, si, :]
                rstd_ops(st, Bst[si], 1.0 / D, xt[:, t, :], Bx, junk, Bj)
                hb, Bh = hbf.next()
                hbs[(ci, t)] = (hb, Bh)
                P.op("dve", lambda e, hb=hb, t=t, st=st, xt=xt: e.scalar_tensor_tensor(
                    out=hb, in0=xt[:, t, :], scalar=st[:, 1:2], in1=g1, op0=ALU.mult, op1=ALU.mult),
                    reads=[Bx, Bst[si], Bg], writes=[Bh])

        def t_stage(ci):
            pb = ci % 2
            hTt, BhT = hT[pb]
            for t in range(4):
                hb, Bh = hbs.pop((ci, t))
                pt_, Bpt = pT.next()
                for k in range(8):
                    P.op("pe", lambda e, pt_=pt_, hb=hb, k=k: e.transpose(pt_[:, k, :], hb[:, k * 128:(k + 1) * 128], ident),
                         reads=[Bh, Bc], writes=[Bpt], inc=(k == 7))
                P.op("act", lambda e, pt_=pt_, t=t, hTt=hTt: e.copy(out=hTt[:, :, t * 128:(t + 1) * 128], in_=pt_),
                     reads=[Bpt], writes=[BhT])

        def j_stage(ci):
            pb = ci % 2
            rt, Br = rp[pb]
            hTt, BhT = hT[pb]
            zt, Bz = zst[pb]
            ut, Bu = ust[pb]
            qt, Bq = qst[pb]
            vt, Bv = vst[pb]
            for cc in range(2):
                pa, Bpa = proj(cc * 128, hTt, BhT)
                pbb, Bpb = proj(256 + cc * 128, hTt, BhT)
                tf, Btf = tmpf.next()
                P.op("act", lambda e, tf=tf, pbb=pbb: e.activation(out=tf, in_=pbb, func=AF.Sigmoid), reads=[Bpb], writes=[Btf])
                P.op("dve", lambda e, zt=zt, cc=cc, pa=pa, tf=tf: e.tensor_tensor(out=zt[:, cc, :], in0=pa, in1=tf, op=ALU.mult),
                     reads=[Bpa, Btf], writes=[Bz])
            for cc in range(2):
                p_, Bp_ = proj(512 + cc * 128, hTt, BhT)
                P.op("act", lambda e, ut=ut, cc=cc, p_=p_: e.copy(out=ut[:, cc, :], in_=p_), reads=[Bp_], writes=[Bu])
            specs = []
            for cc in range(2):
                specs.append((768 + cc * 128, 2304 + cc * 128, 0, [(cc, None)]))
                specs.append((1024 + cc * 128, 2560 + cc * 128, 0, [(2 + cc, 13), (4 + cc, 14)]))
                specs.append((1536 + cc * 128, 2816 + cc * 128, 2, [(6 + cc, None)]))
                specs.append((1792 + cc * 128, 3072 + cc * 128, 2, [(8 + cc, 9), (10 + cc, 10), (12 + cc, 11), (14 + cc, 12)]))
            for col, colP, ri, slots in specs:
                pu, Bpu = proj(col, hTt, BhT)
                ppm, Bppm = proj(colP, hTt, BhT)
                t1, Bt1 = tmpf.next()
                t2, Bt2 = tmpf.next()
                P.op("dve", lambda e, t1=t1, pu=pu, ri=ri, rt=rt: e.tensor_tensor(out=t1, in0=pu, in1=rt[:, ri, :], op=ALU.mult),
                     reads=[Bpu, Br], writes=[Bt1])
                P.op("dve", lambda e, t2=t2, ppm=ppm, ri=ri, rt=rt: e.tensor_tensor(out=t2, in0=ppm, in1=rt[:, ri + 1, :], op=ALU.mult),
                     reads=[Bppm, Br], writes=[Bt2])
                if slots[0][1] is None:
                    s0 = slots[0][0]
                    P.op("dve", lambda e, qt=qt, s0=s0, t1=t1, t2=t2: e.tensor_tensor(out=qt[:, s0, :], in0=t1, in1=t2, op=ALU.add),
                         reads=[Bt1, Bt2], writes=[Bq])
                else:
                    P.op("dve", lambda e, t1=t1, t2=t2: e.tensor_tensor(out=t1, in0=t1, in1=t2, op=ALU.add),
                         reads=[Bt1, Bt2], writes=[Bt1])
                    for s0, mc in slots:
                        P.op("act", lambda e, qt=qt, s0=s0, t1=t1, mc=mc: e.activation(
                            out=qt[:, s0, :], in_=t1, func=AF.Identity, scale=cv[:, mc:mc + 1]),
                            reads=[Bt1, Bg], writes=[Bq])
            for t in range(4):
                p_, Bp_ = pp.next()
                for half, c0 in enumerate((1280, 2048)):
                    for k in range(8):
                        P.op("pe", lambda e, p_=p_, half=half, c0=c0, k=k, t=t, hTt=hTt: e.matmul(
                            p_[:, half * 256:(half + 1) * 256], lhsT=hTt[:, k, t * 128:(t + 1) * 128],
                            rhs=w_sb[:, k, c0:c0 + 256], start=(k == 0), stop=(k == 7)),
                            reads=[Bw, BhT], writes=[Bp_], inc=(half == 1 and k == 7))
                P.op("act", lambda e, vt=vt, t=t, p_=p_: e.copy(out=vt[:, t, :], in_=p_), reads=[Bp_], writes=[Bv])
            cs = slice(ci * 512, (ci + 1) * 512)
            P.dma("sp", zA_d.rearrange("(c p) t -> p c t", p=128)[:, :, cs], zt, reads=[Bz], semkey=f"st{pb}")
            P.dma("sp", uB_d.rearrange("(c p) t -> p c t", p=128)[:, :, cs], ut, reads=[Bu], semkey=f"st{pb}")
            P.dma("sp", qk_d.rearrange("(c p) t -> p c t", p=128)[:, :, cs], qt, reads=[Bq], semkey=f"st{pb}")
            P.dma("sp", v_d[cs, :].rearrange("(t p) c -> p t c", p=128), vt, reads=[Bv], semkey=f"st{pb}")
        n_stage(0)
        t_stage(0)
        for ci in range(8):
            if ci + 1 < 8:
                n_stage(ci + 1)
            j_stage(ci)
            if ci + 1 < 8:
                t_stage(ci + 1)
        P.barrier()
        P.flush()
        sc.close()

    def phase_conv(l):
        sc = Scope(nc)
        zp = sc.sb("zp", [128, 2, 30 + S], BF16)
        Bz = Buf()
        P.op("dve", lambda e: e.memset(zp[:, :, 0:30], 0.0), writes=[Bz])
        P.dma("sp", zp[:, :, 30:30 + S], zA_d.rearrange("(c p) t -> p c t", p=128), writes=[Bz], semkey="ld0")
        dwc = sc.sb("dwc", [128, 2, 31], F32)
        cv = sc.sb("cv", [128, 16], F32)
        pw = sc.sb("pw", [128, 2, G], BF16)
        Bk = Buf()
        P.dma("sp", dwc, dwcol[l].rearrange("p (c j) -> p c j", c=2), writes=[Bk], semkey="ld1")
        P.dma("sp", cv, cvec[l], writes=[Bk], semkey="ld1")
        Bpw = Buf()
        P.dma("pool", pw, conv_pw_w[l].rearrange("(c p) g -> p c g", p=128), writes=[Bpw], semkey="ld2")
        dg = sc.sb("dg", [128, 2, 31, 128], BF16)
        Bdg = Buf()
        for cc in range(2):
            for j in range(31):
                P.op("pool" if j % 2 else "dve", lambda e, cc=cc, j=j: e.tensor_scalar(out=dg[:, cc, j, :], in0=ident, scalar1=dwc[:, cc, j:j + 1],
                                                                   scalar2=None, op0=ALU.mult), reads=[Bc, Bk], writes=[Bdg])
        pc = [[(sc.ps("pc", [128, 512]), Buf()) for _ in range(2)] for _ in range(2)]
        pS1, BS1 = sc.ps("pS1", [128, 512]), Buf()
        pS2, BS2 = sc.ps("pS2", [128, 512]), Buf()
        py = Rot([(sc.ps("py", [128, 512]), Buf()) for _ in range(2)])
        cvs2 = [[(sc.sb("cvs", [128, 512], F32), Buf()) for _ in range(2)] for _ in range(2)]
        sqs2 = [[(sc.sb("sqs", [128, 512], F32), Buf()) for _ in range(2)] for _ in range(2)]
        mean, Bm = sc.sb("mean", [128, 512], F32), Buf()
        msq, Bmsq = sc.sb("msq", [128, 512], F32), Buf()
        var, Bvar = sc.sb("var", [128, 512], F32), Buf()
        dd = [(sc.sb("dd", [128, 512], F32), Buf()) for _ in range(2)]
        zs = [(sc.sb("zs", [128, 512], BF16), Buf()) for _ in range(2)]
        yst = Rot([(sc.sb("yst", [128, 2, 512], BF16), Buf()) for _ in range(2)])
        def c1_stage(ci):
            cvs = cvs2[ci % 2]
            sqs = sqs2[ci % 2]
            for cc in range(2):
                p_, Bp_ = pc[ci % 2][cc]
                for j in range(31):
                    P.op("pe", lambda e, p_=p_, cc=cc, j=j, ci=ci: e.matmul(
                        p_, lhsT=dg[:, cc, j, :], rhs=zp[:, cc, ci * 512 + j: ci * 512 + j + 512],
                        start=(j == 0), stop=(j == 30)), reads=[Bdg, Bz], writes=[Bp_], inc=(j == 30))
                c_, Bc_ = cvs[cc]
                s_, Bs_ = sqs[cc]
                P.op("act", lambda e, c_=c_, p_=p_, cc=cc: e.activation(out=c_, in_=p_, func=AF.Identity, bias=cv[:, cc:cc + 1]),
                     reads=[Bp_, Bk], writes=[Bc_])
                P.op("act", lambda e, s_=s_, p_=p_, cc=cc: e.activation(out=s_, in_=p_, func=AF.Square, bias=cv[:, cc:cc + 1]),
                     reads=[Bp_, Bk], writes=[Bs_])
        def c2_stage(ci):
            cvs = cvs2[ci % 2]
            sqs = sqs2[ci % 2]
            for cc in range(2):
                P.op("pe", lambda e, cc=cc: e.matmul(pS1, lhsT=ones_f, rhs=cvs[cc][0], start=(cc == 0), stop=(cc == 1)),
                     reads=[Bc, cvs[cc][1]], writes=[BS1], inc=(cc == 1))
            for cc in range(2):
                P.op("pe", lambda e, cc=cc: e.matmul(pS2, lhsT=ones_f, rhs=sqs[cc][0], start=(cc == 0), stop=(cc == 1)),
                     reads=[Bc, sqs[cc][1]], writes=[BS2], inc=(cc == 1))
            P.op("dve", lambda e: e.tensor_scalar(out=mean, in0=pS1, scalar1=1.0 / G, scalar2=None, op0=ALU.mult), reads=[BS1], writes=[Bm])
            P.op("dve", lambda e: e.tensor_tensor(out=msq, in0=mean, in1=mean, op=ALU.mult), reads=[Bm], writes=[Bmsq])
            P.op("dve", lambda e: e.scalar_tensor_tensor(out=var, in0=pS2, scalar=1.0 / G, in1=msq, op0=ALU.mult, op1=ALU.subtract),
                 reads=[BS2, Bmsq], writes=[Bvar])
            P.op("dve", lambda e: e.tensor_scalar(out=var, in0=var, scalar1=EPS, scalar2=None, op0=ALU.add), reads=[Bvar], writes=[Bvar])
            P.op("act", lambda e: e.activation(out=var, in_=var, func=AF.Sqrt), reads=[Bvar], writes=[Bvar])
            P.op("dve", lambda e: e.reciprocal(out=var, in_=var), reads=[Bvar], writes=[Bvar])
            for cc in range(2):
                d_, Bd_ = dd[cc]
                z_, Bz_ = zs[cc]
                P.op("dve", lambda e, d_=d_, cc=cc: e.tensor_tensor(out=d_, in0=cvs[cc][0], in1=mean, op=ALU.subtract),
                     reads=[cvs[cc][1], Bm], writes=[Bd_])
                P.op("dve", lambda e, d_=d_: e.tensor_tensor(out=d_, in0=d_, in1=var, op=ALU.mult), reads=[Bd_, Bvar], writes=[Bd_])
                P.op("act", lambda e, d_=d_, z_=z_, cc=cc: e.activation(out=z_, in_=d_, func=AF.Silu, scale=cv[:, 2 + cc:3 + cc],
                                                                       bias=cv[:, 4 + cc:5 + cc]), reads=[Bd_, Bk], writes=[Bz_])
            ys, Bys = yst.next()
            for go in range(2):
                p_, Bp_ = py.next()
                for cc in range(2):
                    P.op("pe", lambda e, p_=p_, go=go, cc=cc: e.matmul(p_, lhsT=pw[:, cc, go * 128:(go + 1) * 128], rhs=zs[cc][0],
                                                                       start=(cc == 0), stop=(cc == 1)),
                         reads=[Bpw, zs[cc][1]], writes=[Bp_], inc=(cc == 1))
                P.op("act", lambda e, ys=ys, go=go, p_=p_: e.copy(out=ys[:, go, :], in_=p_), reads=[Bp_], writes=[Bys])
            P.dma("sp", yT_d.rearrange("(c p) t -> p c t", p=128)[:, 0:2, ci * 512:(ci + 1) * 512], ys, reads=[Bys], semkey=f"st{ci % 2}")

        c1_stage(0)
        for ci in range(8):
            if ci + 1 < 8:
                c1_stage(ci + 1)
            c2_stage(ci)
        P.barrier()
        P.flush()
        sc.close()

    def phase_pool(l):
        sc = Scope(nc)
        up = sc.sb("up", [128, 2, 16 + S], F32)
        Bu = Buf()
        P.op("dve", lambda e: e.memset(up[:, :, 0:16], 0.0), writes=[Bu])
        P.dma("sp", up[:, :, 16:16 + S], uB_d.rearrange("(c p) t -> p c t", p=128), writes=[Bu], semkey="ld0")
        A, BA = sc.sb("A", [128, 16 + S], F32), Buf()
        Bt, BB = sc.sb("Bt", [128, 16 + S], F32), Buf()
        P.op("dve", lambda e: e.memset(A[:, 0:16], 0.0), writes=[BA])
        P.op("dve", lambda e: e.memset(Bt[:, 0:16], 0.0), writes=[BB])
        ybf, By = sc.sb("ybf", [128, 2, S], BF16), Buf()
        t16, Bt16 = sc.sb("t16", [128, 16], F32), Buf()
        bd, Bbd = sc.sb("bd", [128, 2, 128], BF16), Buf()
        cv = sc.sb("cv", [128, 16], F32)
        Bk = Buf()
        P.dma("sp", cv, cvec[l], writes=[Bk], semkey="ld1")
        P.op("pool", lambda e: e.memset(bd, 0.0), writes=[Bbd])
        for g in range(4):
            o = (g % 2) * 64
            P.dma("pool", bd[o:o + 64, g // 2, o:o + 64], pool_w[l, g], writes=[Bbd], semkey="ld2")

        def shadd(dst, Bd, src, Bs, sh):
            P.op("dve", lambda e: e.tensor_tensor(out=dst[:, 16:16 + S], in0=src[:, 16:16 + S], in1=src[:, 16 - sh:16 - sh + S], op=ALU.add),
                 reads=[Bs], writes=[Bd])

        for cc in range(2):
            u = up[:, cc, :]
            shadd(A, BA, u, Bu, 1)
            shadd(Bt, BB, A, BA, 2)
            if cc == 0:
                wl, wh = 2, 4
            else:
                shadd(A, BA, Bt, BB, 4)
                shadd(Bt, BB, A, BA, 8)
                wl, wh = 8, 16
            for (r0, Ssrc, Bs, w) in ((0, A, BA, wl), (64, Bt, BB, wh)):
                rows = slice(r0, r0 + 64)
                P.op("dve", lambda e, rows=rows, Ssrc=Ssrc, w=w, u=u, cc=cc: e.scalar_tensor_tensor(
                    out=ybf[rows, cc, :], in0=Ssrc[rows, 16:16 + S], scalar=1.0 / w, in1=u[rows, 16:16 + S],
                    op0=ALU.mult, op1=ALU.subtract), reads=[Bs, Bu], writes=[By])
                P.op("dve", lambda e, rows=rows, Ssrc=Ssrc, cc=cc: e.tensor_tensor(
                    out=t16[rows, :], in0=Ssrc[rows, 16:32], in1=pooltab[rows, cc, :], op=ALU.mult), reads=[Bs, Bc], writes=[Bt16])
                P.op("dve", lambda e, rows=rows, u=u, cc=cc: e.tensor_tensor(
                    out=ybf[rows, cc, 0:16], in0=t16[rows, :], in1=u[rows, 16:32], op=ALU.subtract), reads=[Bt16, Bu], writes=[By])
        pq = Rot([(sc.ps("pq", [128, 512]), Buf()) for _ in range(4)])
        yst = Rot([(sc.sb("yst", [128, 2, 512], BF16), Buf()) for _ in range(2)])
        for ci in range(8):
            ys, Bys = yst.next()
            for qo in range(2):
                p_, Bp_ = pq.next()
                P.op("pe", lambda e, p_=p_, qo=qo, ci=ci: e.matmul(p_, lhsT=bd[:, qo, :], rhs=ybf[:, qo, ci * 512:(ci + 1) * 512],
                                                                   start=True, stop=True), reads=[Bbd, By], writes=[Bp_])
                P.op("act", lambda e, ys=ys, qo=qo, p_=p_: e.activation(out=ys[:, qo, :], in_=p_, func=AF.Identity,
                                                                       scale=cv[:, 6 + qo:7 + qo]), reads=[Bp_, Bk], writes=[Bys])
            P.dma("sp", yT_d.rearrange("(c p) t -> p c t", p=128)[:, 2:4, ci * 512:(ci + 1) * 512], ys, reads=[Bys], semkey=f"st{ci % 2}")
        P.barrier()
        P.flush()
        sc.close()

    def phase_dil(l):
        sc = Scope(nc)
        qT, Bq = sc.sb("qT", [128, 2, S], BF16), Buf()
        qkv = qk_d.rearrange("(c p) t -> p c t", p=128)
        P.dma("sp", qT, qkv[:, 0:2, :], writes=[Bq], semkey="ld0")
        kTs = []
        for hh_ in range(2):
            kT_, Bk_ = sc.sb("kT", [128, 2, S], BF16), Buf()
            P.dma("sp", kT_, qkv[:, 2 + 2 * hh_:4 + 2 * hh_, :], writes=[Bk_], semkey="ld0")
            kTs.append((kT_, Bk_))
        vs, Bvs = sc.sb("vs", [128, 32, 256], BF16), Buf()
        vE, BvE = sc.sb("vE", [128, 32, 4, 128], BF16), Buf()
        P.op("dve", lambda e: e.memset(vE, 1.0), writes=[BvE])
        Oacc = sc.sb("Oacc", [128, 4, S], F32)
        BO = bufs(2)
        ps_s = Rot([(sc.ps("ps_s", [128, 512]), Buf()) for _ in range(4)])
        ps_o = Rot([(sc.ps("ps_o", [128, 512]), Buf()) for _ in range(3)])
        es = Rot([(sc.sb("es", [128, 512], BF16), Buf()) for _ in range(6)])
        LA = 3
        dslot = {}

        def dA(i, d, r, n, h, nb):
            c, hh = h // 2, h % 2
            rows = slice(0, 128)
            kT, Bk = kTs[hh]
            qv = qT[:, c, :].rearrange("p (n i dd) -> p n i dd", i=128, dd=d)
            kv = kT[:, c, :].rearrange("p (n i dd) -> p n i dd", i=128, dd=d)
            s_, Bs_ = ps_s.next()
            e_, Be_ = es.next()
            dslot[i] = (e_, Be_)
            W = 256 if n > 0 else 128
            P.op("pe", lambda e: e.matmul(s_[:, 0:128], lhsT=kv[rows, n, :, r], rhs=qv[rows, n, :, r], start=True, stop=True),
                 reads=[Bq, Bk], writes=[Bs_], inc=(n == 0))
            if n > 0:
                P.op("pe", lambda e: e.matmul(s_[:, 128:256], lhsT=kv[rows, n - 1, :, r], rhs=qv[rows, n, :, r], start=True, stop=True),
                     reads=[Bq, Bk], writes=[Bs_])
            P.op("act", lambda e: e.activation(out=e_[:, 0:W], in_=s_[:, 0:W], func=AF.Exp, scale=0.125), reads=[Bs_], writes=[Be_])
            P.op("pool" if i % 3 == 2 else "dve", lambda e: e.tensor_tensor(out=e_[:, 0:W], in0=e_[:, 0:W], in1=mask[:, 0:W], op=ALU.mult),
                 reads=[Be_, Bc], writes=[Be_])

        def dB(i, d, r, n, h, nb):
            c = h // 2
            blk = r * nb + n
            e_, Be_ = dslot.pop(i)
            bi = BvE
            vE_ = vE
            o_, Bo_ = ps_o.next()
            P.op("pe", lambda e: e.matmul(o_[:, 0:128], lhsT=vE_[:, blk, h, :], rhs=e_[:, 0:128], start=True, stop=(n == 0)),
                 reads=[bi, Be_], writes=[Bo_], inc=(n == 0))
            if n > 0:
                P.op("pe", lambda e: e.matmul(o_[:, 0:128], lhsT=vE_[:, blk - 1, h, :], rhs=e_[:, 128:256], start=False, stop=True),
                     reads=[bi, Be_], writes=[Bo_])
            ov = Oacc[:, h, :].rearrange("p (n i dd) -> p n i dd", i=128, dd=d)[:, n, :, r]
            o3 = o_[:, 0:128]
            if d == 1:
                P.op("dve", lambda e: e.tensor_copy(out=ov, in_=o3), reads=[Bo_], writes=[BO[c]])
            else:
                P.op("dve", lambda e: e.tensor_tensor(out=ov, in0=ov, in1=o3, op=ALU.add), reads=[Bo_, BO[c]], writes=[BO[c]])

        def run_pipe(dsteps):
            nsteps = len(dsteps)
            for j in range(nsteps + LA):
                if j < nsteps:
                    dA(j, *dsteps[j])
                if j - LA >= 0:
                    dB(j - LA, *dsteps[j - LA])

        import os
        for d in [int(v) for v in os.environ.get('KDIL', '1,4,16').split(',')]:
            nb = 32 // d
            for r in range(d):
                src = v_d[:, 0:256].rearrange("(n jj dd) c -> dd jj n c", jj=128, dd=d)[r]
                P.dma("sp", vs[:, r * nb:(r + 1) * nb, :], src, writes=[Bvs], semkey="ld1")
            for h in range(4):
                off = 0 if h % 2 == 0 else 64
                if h % 2 == 0:
                    P.op("dve", lambda e, h=h, off=off: e.tensor_copy(out=vE[:, :, h, off:off + 64], in_=vs[:, :, h * 64:(h + 1) * 64]),
                         reads=[Bvs], writes=[BvE])
                else:
                    P.op("act", lambda e, h=h, off=off: e.copy(out=vE[:, :, h, off:off + 64], in_=vs[:, :, h * 64:(h + 1) * 64]),
                         reads=[Bvs], writes=[BvE])
            dsteps = []
            for r in range(d):
                for n in range(nb):
                    for h in range(4):
                        dsteps.append((d, r, n, h, nb))
            run_pipe(dsteps)

        tmp, Btmp = sc.sb("tmpr", [128, 1024], F32), Buf()
        yc, Byc = sc.sb("yc", [128, 2, S], BF16), Buf()
        for h in range(4 if not os.environ.get('KNOFIN') else 0):
            c = h // 2
            own = slice(0, 64) if h % 2 == 0 else slice(64, 128)
            oth = slice(64, 128) if h % 2 == 0 else slice(0, 64)
            for q4 in range(4):
                cs = slice(q4 * 1024, (q4 + 1) * 1024)
                P.op("dve", lambda e, own=own, oth=oth, h=h, cs=cs: e.reciprocal(out=tmp[own, :], in_=Oacc[oth, h, cs]), reads=[BO[c]], writes=[Btmp])
                P.op("dve", lambda e, own=own, h=h, c=c, cs=cs: e.tensor_tensor(out=yc[own, c, cs], in0=Oacc[own, h, cs], in1=tmp[own, :], op=ALU.mult),
                     reads=[BO[c], Btmp], writes=[Byc])
        P.dma("sp", yT_d.rearrange("(c p) t -> p c t", p=128)[:, 4:6, :], yc, reads=[Byc], semkey="st0")
        P.barrier()
        P.flush()
        sc.close()

    def phase_diff(l):
        lam_init = 0.8 - 0.6 * math.exp(-0.3 * l)
        sc = Scope(nc)
        qT, Bq = sc.sb("qT", [128, 2, S], BF16), Buf()
        qkv = qk_d.rearrange("(c p) t -> p c t", p=128)
        P.dma("sp", qT, qkv[:, 6:8, :], writes=[Bq], semkey="ld0")
        kvar = {}
        for hh_ in range(2):
            for m_ in range(2):
                kT_, Bk_ = sc.sb("kdT", [128, 2, S], BF16), Buf()
                s0_ = 8 + 2 * (2 * hh_ + m_)
                P.dma("sp", kT_, qkv[:, s0_:s0_ + 2, :], writes=[Bk_], semkey="ld0")
                kvar[(hh_, m_)] = (kT_, Bk_)
        vs, Bvs = sc.sb("vs", [128, 32, 256], BF16), Buf()
        vE, BvE = sc.sb("vE", [128, 32, 4, 128], BF16), Buf()
        P.op("dve", lambda e: e.memset(vE, 1.0), writes=[BvE])
        P.dma("sp", vs, v_d[:, 256:512].rearrange("(n p) c -> p n c", p=128), writes=[Bvs], semkey="ld1")
        for h in range(4):
            off = 0 if h % 2 == 0 else 64
            if h % 2 == 0:
                P.op("dve", lambda e, h=h, off=off: e.tensor_copy(out=vE[:, :, h, off:off + 64], in_=vs[:, :, h * 64:(h + 1) * 64]),
                     reads=[Bvs], writes=[BvE])
            else:
                P.op("act", lambda e, h=h, off=off: e.copy(out=vE[:, :, h, off:off + 64], in_=vs[:, :, h * 64:(h + 1) * 64]),
                     reads=[Bvs], writes=[BvE])
        cv = sc.sb("cv", [128, 16], F32)
        lamb = sc.sb("lamb", [128, 128], F32)
        lst = sc.sb("lst", [128, 8], F32)
        jl = sc.sb("jl", [128, 32], F32)
        Bl = Buf()
        P.dma("sp", cv, cvec[l], writes=[Bl], semkey="ld2")
        P.dma("sp", lamb, diff_lam[l].partition_broadcast(128), writes=[Bl], semkey="ld2")
        for i in range(2):
            P.op("dve", lambda e, i=i: e.scalar_tensor_tensor(out=jl, in0=lamb[:, 64 * i:64 * i + 32], scalar=1.0,
                                                              in1=lamb[:, 64 * i + 32:64 * i + 64], op0=ALU.mult, op1=ALU.mult,
                                                              accum_out=lst[:, i:i + 1]), reads=[Bl], writes=[Bl])
        P.op("act", lambda e: e.activation(out=lst[:, 2:4], in_=lst[:, 0:2], func=AF.Exp), reads=[Bl], writes=[Bl])
        P.op("dve", lambda e: e.tensor_tensor(out=lst[:, 4:5], in0=lst[:, 3:4], in1=lst[:, 2:3], op=ALU.subtract), reads=[Bl], writes=[Bl])
        P.op("dve", lambda e: e.tensor_scalar(out=lst[:, 4:5], in0=lst[:, 4:5], scalar1=-lam_init, scalar2=None, op0=ALU.add),
             reads=[Bl], writes=[Bl])
        ps_s = Rot([(sc.ps("ps_s", [128, 512]), Buf()) for _ in range(4)])
        ps_o = Rot([(sc.ps("ps_o", [128, 512]), Buf()) for _ in range(3)])
        ps_n, Bpn = sc.ps("ps_n", [128, 512]), Buf()
        ps_junk = ps_n
        NDUM = int(os.environ.get("KDUM", "0"))
        NBURST_ALL = os.environ.get("KBALL", "0") == "1"
        es = Rot([(sc.sb("es", [128, 512], BF16), Buf()) for _ in range(6)])
        rl, Brl = sc.sb("rl", [128, 512], F32), Buf()
        om = Rot([(sc.sb("om", [128, 2, 512], F32), Buf()) for _ in range(2)])
        osb = Rot([(sc.sb("osb", [128, 512], F32), Buf()) for _ in range(2)])
        sq, Bsq = sc.sb("sq", [128, 512], F32), Buf()
        rs, Brs = sc.sb("rs", [128, 512], F32), Buf()
        yst = Rot([(sc.sb("yst", [128, 512], BF16), Buf()) for _ in range(2)])
        scale = 32.0 ** -0.5
        LA = 3
        steps = []
        slot = {}

        def mkA(i, c, qc, hh, m, kb):
            def A():
                rows = slice(0, 128)
                kT, Bk = kvar[(hh, m)]
                off = max(0, 128 * kb - 512 * qc)
                s_, Bs_ = ps_s.next()
                e_, Be_ = es.next()
                slot[i] = (e_, Be_)
                P.op("pe", lambda e: e.matmul(
                    s_[:, off:512], lhsT=kT[rows, c, kb * 128:(kb + 1) * 128], rhs=qT[rows, c, qc * 512 + off:(qc + 1) * 512],
                    start=True, stop=True), reads=[Bk, Bq], writes=[Bs_])
                if kb == 0 and (NBURST_ALL or (hh == 0 and m == 0)):
                    for _ in range(NDUM):
                        P.op("pe", lambda e: e.matmul(ps_junk, lhsT=ones_bf, rhs=mask, start=True, stop=True), inc=False)
                P.op("act", lambda e: e.activation(out=e_[:, off:512], in_=s_[:, off:512], func=AF.Exp, scale=scale),
                     reads=[Bs_], writes=[Be_])
                if kb >= 4 * qc:
                    P.op("pool", lambda e: e.tensor_tensor(out=e_[:, off:off + 128], in0=e_[:, off:off + 128],
                                                           in1=mask[:, 0:128], op=ALU.mult), reads=[Be_, Bc], writes=[Be_])
            return A

        grp = {}

        def mkB(i, c, qc, hh, m, kb):
            def B():
                rows = slice(hh * 64, hh * 64 + 64)
                oth = slice((1 - hh) * 64, (1 - hh) * 64 + 64)
                h = 2 * c + hh
                nkb = 4 * qc + 4
                off = max(0, 128 * kb - 512 * qc)
                if kb == 0:
                    grp["o"] = ps_o.next()
                    if hh == 0 and m == 0:
                        grp["om"] = om.next()
                o_, Bo_ = grp["o"]
                om_, Bom = grp["om"]
                e_, Be_ = slot.pop(i)
                P.op("pe", lambda e: e.matmul(o_[:, off:512], lhsT=vE[:, kb, h, :], rhs=e_[:, off:512],
                                              start=(kb == 0), stop=(kb == nkb - 1)),
                     reads=[BvE, Be_], writes=[Bo_], inc=(kb == nkb - 1))
                if kb < nkb - 1:
                    return
                P.op("dve", lambda e: e.reciprocal(out=rl[rows, :], in_=o_[oth, :]), reads=[Bo_], writes=[Brl])
                P.op("dve", lambda e: e.tensor_tensor(out=om_[rows, m, :], in0=o_[rows, :], in1=rl[rows, :], op=ALU.mult),
                     reads=[Bo_, Brl], writes=[Bom])
                if not (hh == 1 and m == 1):
                    return
                ob, Bob = osb.next()
                P.op("dve", lambda e: e.scalar_tensor_tensor(out=ob, in0=om_[:, 1, :], scalar=lst[:, 4:5], in1=om_[:, 0, :],
                                                             op0=ALU.mult, op1=ALU.add), reads=[Bom, Bl], writes=[Bob])
                P.op("act", lambda e: e.activation(out=sq, in_=ob, func=AF.Square), reads=[Bob], writes=[Bsq])
                P.op("pe", lambda e: e.matmul(ps_n, lhsT=blockones, rhs=sq, start=True, stop=True), reads=[Bc, Bsq], writes=[Bpn])
                P.op("dve", lambda e: e.tensor_scalar(out=rs, in0=ps_n, scalar1=1.0 / 64, scalar2=EPS, op0=ALU.mult, op1=ALU.add),
                     reads=[Bpn], writes=[Brs])
                P.op("act", lambda e: e.activation(out=rs, in_=rs, func=AF.Sqrt), reads=[Brs], writes=[Brs])
                P.op("dve", lambda e: e.reciprocal(out=rs, in_=rs), reads=[Brs], writes=[Brs])
                P.op("dve", lambda e: e.tensor_tensor(out=ob, in0=ob, in1=rs, op=ALU.mult), reads=[Bob, Brs], writes=[Bob])
                ys, Bys = yst.next()
                P.op("dve", lambda e: e.tensor_scalar(out=ys, in0=ob, scalar1=cv[:, 8:9], scalar2=(1.0 - lam_init),
                                                      op0=ALU.mult, op1=ALU.mult), reads=[Bob, Bl], writes=[Bys])
                P.dma("sp", yT_d[(6 + c) * 128:(7 + c) * 128, qc * 512:(qc + 1) * 512], ys, reads=[Bys])
            return B

        i = 0
        for c in range(2):
            for qc in range(8):
                for hh in range(2):
                    for m in range(2):
                        for kb in range(4 * qc + 4):
                            steps.append((mkA(i, c, qc, hh, m, kb), mkB(i, c, qc, hh, m, kb)))
                            i += 1
        n = len(steps)
        for j in range(n + LA):
            if j < n:
                steps[j][0]()
            if j - LA >= 0:
                steps[j - LA][1]()
        P.barrier()
        P.flush()
        sc.close()

    def phase_ffn(l, x_src):
        moe = (l % 2 == 1)
        last = (l == 1)
        osc = Scope(nc)
        yacc = osc.sb("yacc", [128, 16, D], F32)
        By = bufs(16)
        h2T = osc.sb("h2T", [128, 8, 2048], BF16)
        Bh = bufs(4)
        gates = osc.sb("gates", [128, 16, 8], F32)
        Bgt = bufs(16)
        wo = osc.sb("wo", [128, 8, D], BF16)
        Bwo = Buf()
        for k in range(8):
            P.dma("pool", wo[:, k, :], w_out[l, k * 128:(k + 1) * 128, :], writes=[Bwo], semkey="w")
        for ps_ in range(2):
            sc = Scope(nc)
            g2 = sc.sb("g2", [128, D], F32)
            Bg = Buf()
            P.dma("sp", g2, norm2_g[l:l + 1, :].partition_broadcast(128), writes=[Bg], semkey="c")
            if moe:
                rB = sc.sb("rB", [128, NE, D], F32)
                P.dma("sp", rB, routerT.partition_broadcast(128).rearrange("p o (e d) -> p (o e) d", e=NE), writes=[Bg], semkey="c")
                h2f = Rot([(sc.sb("h2f", [128, D], F32), Buf()) for _ in range(2)])
                jf, Bjf = sc.sb("jf", [128, D], F32), Buf()
                lg = sc.sb("lg", [128, 16, 8], F32)
                sm = sc.sb("sm", [128, 16, 32], F32)
                Blg = bufs(16)
            xs = Rot([(sc.sb("xs", [128, D], F32), Buf()) for _ in range(4)])
            yTs = Rot([(sc.sb("yTs", [128, 8, 128], BF16), Buf()) for _ in range(4)])
            hbf = Rot([(sc.sb("hbf", [128, D], BF16), Buf()) for _ in range(3)])
            junk, Bj = sc.sb("junk", [128, D], BF16), Buf()
            stat = sc.sb("stat", [128, 16, 2], F32)
            Bst = bufs(16)
            po = Rot([(sc.ps("po", [128, D]), Buf()) for _ in range(3)])
            pT = Rot([(sc.ps("pT", [128, 8, 128], BF16), Buf()) for _ in range(2)])
            wres = {}
            nres = {}

            def w_stage(tt):
                gt = ps_ * 16 + tt
                xt, Bx = xs.next()
                yt, Byt = yTs.next()
                P.dma("sp", xt, x_src[gt * 128:(gt + 1) * 128, :], writes=[Bx], semkey=f"x{tt % 2}")
                P.dma("sp", yt, yT_d.rearrange("(k p) t -> p k t", p=128)[:, :, gt * 128:(gt + 1) * 128], writes=[Byt], semkey=f"x{tt % 2}")
                p_, Bp_ = po.next()
                for half in range(2):
                    for k in range(8):
                        P.op("pe", lambda e, p_=p_, half=half, k=k, yt=yt: e.matmul(
                            p_[:, half * 512:(half + 1) * 512], lhsT=yt[:, k, :], rhs=wo[:, k, half * 512:(half + 1) * 512],
                            start=(k == 0), stop=(k == 7)), reads=[Byt, Bwo], writes=[Bp_], inc=(half == 1 and k == 7))
                wres[tt] = (xt, Bx, p_, Bp_)

            def n_stage(tt):
                xt, Bx, p_, Bp_ = wres.pop(tt)
                ya = yacc[:, tt, :]
                P.op("dve", lambda e, ya=ya, p_=p_, xt=xt: e.tensor_tensor(out=ya, in0=p_, in1=xt, op=ALU.add), reads=[Bp_, Bx], writes=[By[tt]])
                st = stat[:, tt, :]
                rstd_ops(st, Bst[tt], 1.0 / D, ya, By[tt], junk, Bj)
                hb, Bhb = hbf.next()
                if not moe:
                    P.op("dve", lambda e, hb=hb, ya=ya, st=st: e.scalar_tensor_tensor(out=hb, in0=ya, scalar=st[:, 1:2], in1=g2,
                                                                                     op0=ALU.mult, op1=ALU.mult),
                         reads=[By[tt], Bst[tt], Bg], writes=[Bhb])
                else:
                    hf, Bhf = h2f.next()
                    P.op("dve", lambda e, hf=hf, ya=ya, st=st: e.scalar_tensor_tensor(out=hf, in0=ya, scalar=st[:, 1:2], in1=g2,
                                                                                     op0=ALU.mult, op1=ALU.mult),
                         reads=[By[tt], Bst[tt], Bg], writes=[Bhf])
                    P.op("act", lambda e, hb=hb, hf=hf: e.copy(out=hb, in_=hf), reads=[Bhf], writes=[Bhb])
                    L = lg[:, tt, :]
                    BL = Blg[tt]
                    for ex in range(NE):
                        P.op("dve", lambda e, hf=hf, ex=ex, L=L: e.scalar_tensor_tensor(
                            out=jf, in0=hf, scalar=1.0, in1=rB[:, ex, :], op0=ALU.mult, op1=ALU.mult, accum_out=L[:, ex:ex + 1]),
                            reads=[Bhf, Bg], writes=[Bjf, BL])
                    m = sm[:, tt, :]
                    P.op("dve", lambda e, m=m, L=L: e.tensor_reduce(out=m[:, 0:1], in_=L, axis=AX.X, op=ALU.max), reads=[BL], writes=[BL])
                    P.op("dve", lambda e, m=m, L=L: e.tensor_scalar(out=m[:, 8:16], in0=L, scalar1=m[:, 0:1], scalar2=None, op0=ALU.is_equal),
                         reads=[BL], writes=[BL])
                    P.op("dve", lambda e, m=m, L=L: e.scalar_tensor_tensor(out=m[:, 16:24], in0=m[:, 8:16], scalar=-1e30, in1=L,
                                                                          op0=ALU.mult, op1=ALU.add), reads=[BL], writes=[BL])
                    P.op("dve", lambda e, m=m: e.tensor_reduce(out=m[:, 1:2], in_=m[:, 16:24], axis=AX.X, op=ALU.max), reads=[BL], writes=[BL])
                    P.op("dve", lambda e, m=m: e.tensor_scalar(out=m[:, 16:24], in0=m[:, 16:24], scalar1=m[:, 1:2], scalar2=None, op0=ALU.is_equal),
                         reads=[BL], writes=[BL])
                    P.op("dve", lambda e, m=m: e.tensor_tensor(out=m[:, 2:3], in0=m[:, 1:2], in1=m[:, 0:1], op=ALU.subtract), reads=[BL], writes=[BL])
                    P.op("act", lambda e, m=m: e.activation(out=m[:, 2:3], in_=m[:, 2:3], func=AF.Exp), reads=[BL], writes=[BL])
                    P.op("dve", lambda e, m=m: e.tensor_scalar(out=m[:, 3:4], in0=m[:, 2:3], scalar1=1.0, scalar2=None, op0=ALU.add), reads=[BL], writes=[BL])
                    P.op("dve", lambda e, m=m: e.reciprocal(out=m[:, 3:4], in_=m[:, 3:4]), reads=[BL], writes=[BL])
                    P.op("dve", lambda e, m=m: e.tensor_tensor(out=m[:, 4:5], in0=m[:, 2:3], in1=m[:, 3:4], op=ALU.mult), reads=[BL], writes=[BL])
                    P.op("dve", lambda e, m=m: e.tensor_scalar(out=m[:, 24:32], in0=m[:, 8:16], scalar1=m[:, 3:4], scalar2=None, op0=ALU.mult),
                         reads=[BL], writes=[BL])
                    P.op("dve", lambda e, m=m, tt=tt: e.scalar_tensor_tensor(out=gates[:, tt, :], in0=m[:, 16:24], scalar=m[:, 4:5], in1=m[:, 24:32],
                                                                            op0=ALU.mult, op1=ALU.add), reads=[BL], writes=[Bgt[tt]])
                nres[tt] = (hb, Bhb)

            def t_stage(tt):
                hb, Bhb = nres.pop(tt)
                pt_, Bpt = pT.next()
                for k in range(8):
                    P.op("pe", lambda e, pt_=pt_, hb=hb, k=k: e.transpose(pt_[:, k, :], hb[:, k * 128:(k + 1) * 128], ident),
                         reads=[Bhb, Bc], writes=[Bpt], inc=(k == 7))
                P.op("act", lambda e, pt_=pt_, tt=tt: e.copy(out=h2T[:, :, tt * 128:(tt + 1) * 128], in_=pt_), reads=[Bpt], writes=[Bh[tt // 4]])

            LA3 = 2
            for tt in range(min(LA3, 16)):
                w_stage(tt)
            for tt in range(16):
                if tt + LA3 < 16:
                    w_stage(tt + LA3)
                n_stage(tt)
                t_stage(tt)
            P.barrier()
            P.flush()
            sc.close()
            sc = Scope(nc)
            wg = [(sc.sb("wg", [128, 8, 512], BF16), Buf()) for _ in range(2)]
            wu = [(sc.sb("wu", [128, 8, 512], BF16), Buf()) for _ in range(2)]
            wd = [(sc.sb("wd", [128, 4, D], BF16), Buf()) for _ in range(2)]
            act = Rot([(sc.sb("act", [128, 4, 512], BF16), Buf()) for _ in range(3)])
            sg = Rot([(sc.sb("sg", [128, 512], F32), Buf()) for _ in range(3)])
            pg = Rot([(sc.ps("pg", [128, 512]), Buf()) for _ in range(2)])
            pu = Rot([(sc.ps("pu", [128, 512]), Buf()) for _ in range(2)])
            pd = Rot([(sc.ps("pd", [128, D]), Buf()) for _ in range(2)])
            slabs = []
            if not moe:
                f0 = 0
                while f0 < DFF:
                    fw = min(512, DFF - f0)
                    slabs.append((ffn_wg[0], ffn_wu[0], ffn_wd[0], None, f0, fw))
                    f0 += fw
            else:
                for ex in range(NE):
                    for f0 in range(0, DFE, 512):
                        slabs.append((moe_wg[0, ex], moe_wu[0, ex], moe_wd[0, ex], ex, f0, 512))

            def load(i):
                g_, u_, d_, ex, f0, fw = slabs[i]
                b = i % 2
                P.dma("pool", wg[b][0][:, :, 0:fw], g_[:, f0:f0 + fw].rearrange("(k p) f -> p k f", p=128), writes=[wg[b][1]], semkey=f"wg{b}")
                P.dma("pool", wu[b][0][:, :, 0:fw], u_[:, f0:f0 + fw].rearrange("(k p) f -> p k f", p=128), writes=[wu[b][1]], semkey=f"wu{b}")
                P.dma("pool", wd[b][0][:, 0:fw // 128, :], d_[f0:f0 + fw, :].rearrange("(k p) n -> p k n", p=128), writes=[wd[b][1]], semkey=f"wd{b}")

            acts = {}

            def gu_stage(i, cq):
                g_, u_, d_, ex, f0, fw = slabs[i]
                b = i % 2
                nfc = fw // 128
                wgt, Bwg = wg[b]
                wut, Bwu = wu[b]
                a_, Ba_ = act.next()
                acts[(i, cq)] = (a_, Ba_)
                for fc in range(nfc):
                    pg_, Bpg = pg.next()
                    pu_, Bpu = pu.next()
                    for (pz, Bpz, wz, Bwz) in ((pg_, Bpg, wgt, Bwg), (pu_, Bpu, wut, Bwu)):
                        for k in range(8):
                            P.op("pe", lambda e, pz=pz, wz=wz, k=k, fc=fc, cq=cq: e.matmul(
                                pz, lhsT=wz[:, k, fc * 128:(fc + 1) * 128], rhs=h2T[:, k, cq * 512:(cq + 1) * 512],
                                start=(k == 0), stop=(k == 7)), reads=[Bwz, Bh[cq]], writes=[Bpz], inc=(k == 7))
                    s_, Bs_ = sg.next()
                    P.op("act", lambda e, s_=s_, pg_=pg_: e.activation(out=s_, in_=pg_, func=AF.Silu), reads=[Bpg], writes=[Bs_])
                    P.op("dve", lambda e, a_=a_, fc=fc, pu_=pu_, s_=s_: e.tensor_tensor(out=a_[:, fc, :], in0=pu_, in1=s_, op=ALU.mult),
                         reads=[Bpu, Bs_], writes=[Ba_])

            def d_stage(i, cq):
                g_, u_, d_, ex, f0, fw = slabs[i]
                b = i % 2
                nfc = fw // 128
                wdt, Bwd = wd[b]
                a_, Ba_ = acts.pop((i, cq))
                for t in range(4):
                    tt = cq * 4 + t
                    pd_, Bpd = pd.next()
                    for half in range(2):
                        for fc in range(nfc):
                            P.op("pe", lambda e, pd_=pd_, half=half, fc=fc, a_=a_, t=t, wdt=wdt, nfc=nfc: e.matmul(
                                pd_[:, half * 512:(half + 1) * 512], lhsT=a_[:, fc, t * 128:(t + 1) * 128],
                                rhs=wdt[:, fc, half * 512:(half + 1) * 512], start=(fc == 0), stop=(fc == nfc - 1)),
                                reads=[Ba_, Bwd], writes=[Bpd], inc=(half == 1 and fc == nfc - 1))
                    ya = yacc[:, tt, :]
                    if ex is None:
                        P.op("dve", lambda e, ya=ya, pd_=pd_: e.tensor_tensor(out=ya, in0=pd_, in1=ya, op=ALU.add), reads=[Bpd, By[tt]], writes=[By[tt]])
                    else:
                        P.op("dve", lambda e, ya=ya, pd_=pd_, tt=tt, ex=ex: e.scalar_tensor_tensor(
                            out=ya, in0=pd_, scalar=gates[:, tt, ex:ex + 1], in1=ya, op0=ALU.mult, op1=ALU.add),
                            reads=[Bpd, By[tt], Bgt[tt]], writes=[By[tt]])

            units = [(i, cq) for i in range(len(slabs)) for cq in range(4)]
            load(0)
            if len(slabs) > 1:
                load(1)
            gu_stage(*units[0])
            for u in range(len(units)):
                if u + 1 < len(units):
                    gu_stage(*units[u + 1])
                d_stage(*units[u])
                i, cq = units[u]
                if cq == 3 and i + 2 < len(slabs):
                    load(i + 2)
            P.barrier()
            P.flush()
            sc.close()
            sc = Scope(nc)
            if not last:
                for q4 in range(4):
                    P.dma("sp", xmid.rearrange("(n p) d -> p n d", p=128)[:, ps_ * 16 + q4 * 4: ps_ * 16 + q4 * 4 + 4, :],
                          yacc[:, q4 * 4:(q4 + 1) * 4, :], reads=By[q4 * 4:(q4 + 1) * 4], semkey="xo")
            else:
                gf = sc.sb("gf", [128, D], F32)
                Bgf = Buf()
                P.dma("sp", gf, final_g.partition_broadcast(128), writes=[Bgf], semkey="c")
                junk, Bj = sc.sb("junk", [128, D], BF16), Buf()
                stat = sc.sb("stat", [128, 16, 2], F32)
                Bst = bufs(16)
                ost = Rot([(sc.sb("ost", [128, D], F32), Buf()) for _ in range(3)])
                for tt in range(16):
                    ya = yacc[:, tt, :]
                    st = stat[:, tt, :]
                    rstd_ops(st, Bst[tt], 1.0 / D, ya, By[tt], junk, Bj)
                    o_, Bo_ = ost.next()
                    P.op("dve", lambda e, o_=o_, ya=ya, st=st: e.scalar_tensor_tensor(out=o_, in0=ya, scalar=st[:, 1:2], in1=gf,
                                                                                     op0=ALU.mult, op1=ALU.mult),
                         reads=[By[tt], Bst[tt], Bgf], writes=[Bo_])
                    gt = ps_ * 16 + tt
                    P.dma("sp", out[gt * 128:(gt + 1) * 128, :], o_, reads=[Bo_], semkey=f"xo{tt % 3}")
            P.barrier()
            P.flush()
            sc.close()
        osc.close()

    for l in range(nlayers):
        x_src = x_in if l == 0 else xmid
        for nm, fn in (("p1", lambda: phase_p1(l, x_src)), ("conv", lambda: phase_conv(l)), ("pool", lambda: phase_pool(l)),
                       ("dil", lambda: phase_dil(l)), ("diff", lambda: phase_diff(l)), ("ffn", lambda: phase_ffn(l, x_src))):
            if only is None or nm in only:
                fn()
    P.barrier()
    P.flush()
    gs.close()
    return nc, P


def _rope_np(seq, rot):
    pos = np.arange(seq, dtype=np.float32)
    inv = (np.float32(THETA) ** (-(np.arange(0, rot, 2, dtype=np.float32)) / np.float32(rot))).astype(np.float32)
    ang = (pos[:, None] * inv[None, :]).astype(np.float32)
    return np.cos(ang).astype(np.float32), np.sin(ang).astype(np.float32)


def _consts():
    cos_c, sin_c = _rope_np(S, 16)
    cos_d, sin_d = _rope_np(S, 8)
    rope = np.zeros((4, 128, S), np.float32)
    for p in range(128):
        i = p % 64
        if i < 8:
            rope[0, p] = cos_c[:, i]
            rope[1, p] = -sin_c[:, i]
        elif i < 16:
            rope[0, p] = cos_c[:, i - 8]
            rope[1, p] = sin_c[:, i - 8]
        else:
            rope[0, p] = 1.0
        i = p % 32
        if i < 4:
            rope[2, p] = cos_d[:, i]
            rope[3, p] = -sin_d[:, i]
        elif i < 8:
            rope[2, p] = cos_d[:, i - 4]
            rope[3, p] = sin_d[:, i - 4]
        else:
            rope[2, p] = 1.0
    cst = np.zeros((128, 928), np.float32)
    cst[:, 0:128] = np.eye(128, dtype=np.float32)
    cst[0:64, 128:192] = 1.0
    cst[64:128, 192:256] = 1.0
    cst[:, 256:384] = 1.0
    jj = np.arange(128)[:, None]
    ii = np.arange(128)[None, :]
    m = np.concatenate([(jj <= ii), (jj >= ii)], axis=1).astype(np.float32)
    cst[:, 384:640] = m
    cst[:, 640:896] = m
    t = np.arange(16, dtype=np.float32)
    for c in range(2):
        for p in range(128):
            w = (2, 4, 8, 16)[c * 2 + p // 64]
            cst[p, 896 + c * 16: 896 + (c + 1) * 16] = 1.0 / np.minimum(t + 1.0, float(w))
    return rope, cst


def _perm_cols():
    idx = []
    for base, hd, half in ((3 * G, 64, 8), (4 * G, 64, 8), (6 * G, 32, 4), (7 * G, 32, 4)):
        for j in range(G):
            i = j % hd
            if i < half:
                pj = j + half
            elif i < 2 * half:
                pj = j - half
            else:
                pj = j
            idx.append(base + pj)
    return np.asarray(idx)


_CACHE = {}


def kernel(x, norm1_g, w_in, conv_dw_w, conv_dw_b, conv_ln_g, conv_ln_b, conv_pw_w,
           pool_w, pool_scale, diff_lam, diff_ln_g, w_out, norm2_g,
           ffn_w_gate, ffn_w_up, ffn_w_down, moe_router, moe_w_gate, moe_w_up, moe_w_down,
           final_g):
    f = lambda a: np.ascontiguousarray(np.asarray(a, dtype=np.float32))
    if "nc" not in _CACHE:
        _CACHE["nc"] = build_program()[0]
        _CACHE["consts"] = _consts()
    nc = _CACHE["nc"]
    rope, cst = _CACHE["consts"]
    w_in = f(w_in)
    w_inP = np.ascontiguousarray(w_in[:, :, _perm_cols()])
    col = lambda v: np.asarray(v, np.float32).reshape(2, 2, 128).transpose(0, 2, 1)
    cvec = np.zeros((2, 128, 16), np.float32)
    cvec[:, :, 0:2] = col(conv_dw_b)
    cvec[:, :, 2:4] = col(conv_ln_g)
    cvec[:, :, 4:6] = col(conv_ln_b)
    cvec[:, :, 6:8] = col(pool_scale)
    cvec[:, :, 8] = np.concatenate([np.asarray(diff_ln_g, np.float32)] * 2, axis=1)
    pp = np.arange(128)
    for j in range(4):
        cvec[:, :, 9 + j] = ((pp // 32) == j).astype(np.float32)
    for j in range(2):
        cvec[:, :, 13 + j] = ((pp // 64) == j).astype(np.float32)
    dwcol = np.ascontiguousarray(np.asarray(conv_dw_w, np.float32).reshape(2, 31, 2, 128).transpose(0, 3, 2, 1)).reshape(2, 128, 62)
    shared = {
        "norm1_g": f(norm1_g), "w_in": w_in, "w_inP": w_inP, "dwcol": dwcol, "cvec": cvec,
        "conv_pw_w": f(conv_pw_w), "pool_w": f(pool_w), "diff_lam": f(diff_lam).reshape(2, 1, 128),
        "w_out": f(w_out), "norm2_g": f(norm2_g), "ffn_w_gate": f(ffn_w_gate), "ffn_w_up": f(ffn_w_up),
        "ffn_w_down": f(ffn_w_down), "routerT": np.ascontiguousarray(f(moe_router)[0].T).reshape(1, NE * D),
        "moe_w_gate": f(moe_w_gate), "moe_w_up": f(moe_w_up), "moe_w_down": f(moe_w_down),
        "final_g": f(final_g).reshape(1, D), "ropeT": rope, "cst": cst,
    }
    x = f(x)
    in_maps = [dict(shared, x=x[b]) for b in range(8)]
    res = run_bass_kernel_spmd(nc, in_maps, core_ids=list(range(8)))
    return np.stack([np.asarray(r["out"], dtype=np.float32) for r in res.results], axis=0)
```
